# Optimizing a Trainium2 kernel written in Bass

```python
import math
import jax, jax.numpy as jnp
from jax import lax
import numpy as np

D_MODEL = 4096
BATCH = 2
SEQ = 8192
DEPTH = 2

RMS_EPS = 1e-6
GDN_HEADS = D_MODEL // 512
GDN_HEAD_DIM = 128
GDN_WIDTH = GDN_HEADS * GDN_HEAD_DIM
GDN_CONV_WIDTH = 4
GDN_CHUNK = 64
CONF_CHANNELS = D_MODEL // 4
CONF_KERNEL = 31
DIFF_HEADS = D_MODEL // 512
DIFF_QK_DIM = 64
DIFF_V_DIM = 2 * DIFF_QK_DIM
DIFF_QK_WIDTH = DIFF_HEADS * 2 * DIFF_QK_DIM
DIFF_V_WIDTH = DIFF_HEADS * DIFF_V_DIM
Q_BLOCK = 128
N_EXPERTS = 16
N_GROUPS = 4
TOP_K = 2
EXPERT_FF = D_MODEL // 4
ADA_CHUNKS = 6
IN_SIZES = (GDN_WIDTH, GDN_WIDTH, GDN_WIDTH, GDN_WIDTH, GDN_HEADS, GDN_HEADS,
            2 * CONF_CHANNELS,
            DIFF_QK_WIDTH, DIFF_QK_WIDTH, DIFF_V_WIDTH,
            D_MODEL, D_MODEL, D_MODEL)
IN_COLS = sum(IN_SIZES)

kernel_name = 'hybrid_gdn_conformer_diffattn_grouped_moe'


def rmsnorm(x, g):
    xf = x.astype(jnp.float32)
    y = xf * lax.rsqrt(jnp.mean(xf * xf, axis=-1, keepdims=True) + RMS_EPS)
    return (y * g.astype(jnp.float32)).astype(x.dtype)


def layernorm(x, g, b):
    xf = x.astype(jnp.float32)
    mu = jnp.mean(xf, axis=-1, keepdims=True)
    xc = xf - mu
    var = jnp.mean(xc * xc, axis=-1, keepdims=True)
    return (xc * lax.rsqrt(var + RMS_EPS) * g.astype(jnp.float32) + b.astype(jnp.float32)).astype(x.dtype)


def l2norm(x):
    xf = x.astype(jnp.float32)
    return xf * lax.rsqrt(jnp.sum(xf * xf, axis=-1, keepdims=True) + RMS_EPS)


def causal_depthwise_conv(x, w):
    k_w, ch = w.shape
    return lax.conv_general_dilated(
        x, w[:, None, :].astype(x.dtype), window_strides=(1,), padding=[(k_w - 1, 0)],
        dimension_numbers=('NWC', 'WIO', 'NWC'), feature_group_count=ch)


def split_columns(p):
    out = []
    start = 0
    for size in IN_SIZES:
        out.append(p[..., start:start + size])
        start += size
    return out


def alibi_slopes(n_heads):
    return 2.0 ** (-8.0 * jnp.arange(1, n_heads + 1, dtype=jnp.float32) / n_heads)


def chunked_gated_delta_rule(q, k, v, g, beta):
    B, T, H, Dk = q.shape
    Dv = v.shape[-1]
    C = GDN_CHUNK
    N = T // C
    f32 = jnp.float32

    def chunks(t):
        return t.astype(f32).reshape(B, N, C, H, -1).transpose(0, 3, 1, 2, 4)

    q = chunks(q) * (Dk ** -0.5)
    k = chunks(k)
    v = chunks(v)
    g = jnp.cumsum(chunks(g[..., None])[..., 0], axis=-1)
    beta = chunks(beta[..., None])[..., 0]
    causal = jnp.tril(jnp.ones((C, C), bool))
    strict = jnp.tril(jnp.ones((C, C), bool), -1)
    gdiff = g[..., :, None] - g[..., None, :]
    decay = jnp.where(causal, jnp.exp(jnp.where(causal, gdiff, 0.0)), 0.0)
    k_beta = k * beta[..., None]
    lower = jnp.where(strict, jnp.einsum('bhnid,bhnjd->bhnij', k_beta, k) * decay, 0.0)
    eye = jnp.eye(C, dtype=f32)
    t_inv = lax.linalg.triangular_solve(eye + lower, jnp.broadcast_to(eye, lower.shape),
                                        left_side=True, lower=True, unit_diagonal=True)
    u = jnp.einsum('bhnij,bhnjd->bhnid', t_inv, v * beta[..., None])
    w = jnp.einsum('bhnij,bhnjd->bhnid', t_inv, k_beta * jnp.exp(g)[..., None])
    qk = jnp.einsum('bhnid,bhnjd->bhnij', q, k) * decay
    q_dec = q * jnp.exp(g)[..., None]
    k_tail = k * jnp.exp(g[..., -1:] - g)[..., None]
    chunk_decay = jnp.exp(g[..., -1])

    def step(S, xs):
        q_i, k_i, u_i, w_i, qk_i, d_i = xs
        v_new = u_i - jnp.einsum('bhcd,bhde->bhce', w_i, S)
        o_i = jnp.einsum('bhcd,bhde->bhce', q_i, S) + jnp.einsum('bhij,bhje->bhie', qk_i, v_new)
        S = S * d_i[..., None, None] + jnp.einsum('bhcd,bhce->bhde', k_i, v_new)
        return S, o_i

    xs = tuple(jnp.moveaxis(t, 2, 0) for t in (q_dec, k_tail, u, w, qk, chunk_decay))
    S0 = jnp.zeros((B, H, Dk, Dv), f32)
    _, o = lax.scan(step, S0, xs)
    return o.transpose(1, 0, 3, 2, 4).reshape(B, T, H, Dv)


def gdn_mixer(q, k, v, z, b, a, conv_w, a_log, dt_bias, norm_g, w_out):
    B, T, _ = q.shape
    H, hd = GDN_HEADS, GDN_HEAD_DIM
    qkv = jax.nn.silu(causal_depthwise_conv(jnp.concatenate([q, k, v], axis=-1), conv_w))
    q, k, v = qkv[..., :GDN_WIDTH], qkv[..., GDN_WIDTH:2 * GDN_WIDTH], qkv[..., 2 * GDN_WIDTH:]
    q = l2norm(q.reshape(B, T, H, hd))
    k = l2norm(k.reshape(B, T, H, hd))
    v = v.reshape(B, T, H, hd)
    beta = jax.nn.sigmoid(b.astype(jnp.float32))
    g = -jnp.exp(a_log.astype(jnp.float32)) * jax.nn.softplus(a.astype(jnp.float32) + dt_bias.astype(jnp.float32))
    o = chunked_gated_delta_rule(q, k, v, g, beta)
    o = rmsnorm(o, norm_g) * jax.nn.silu(z.reshape(B, T, H, hd).astype(jnp.float32))
    return o.reshape(B, T, GDN_WIDTH) @ w_out


def conformer_conv_mixer(u, dw_w, dw_b, ln_g, ln_b, w_out):
    h = jax.nn.glu(u, axis=-1)
    h = causal_depthwise_conv(h, dw_w) + dw_b
    h = jax.nn.silu(layernorm(h, ln_g, ln_b))
    return h @ w_out


def diff_attention_mixer(q, k, v, q_norm_g, k_norm_g, lam_q1, lam_k1, lam_q2, lam_k2,
                         sub_g, w_out, lambda_init):
    B, T, _ = q.shape
    H, d, dv = DIFF_HEADS, DIFF_QK_DIM, DIFF_V_DIM
    f32 = jnp.float32
    q = rmsnorm(q.reshape(B, T, H, 2, d), q_norm_g).astype(f32) * (d ** -0.5)
    k = rmsnorm(k.reshape(B, T, H, 2, d), k_norm_g).astype(f32)
    v = v.reshape(B, T, H, dv).astype(f32)
    lam = (jnp.exp(jnp.sum(lam_q1.astype(f32) * lam_k1.astype(f32)))
           - jnp.exp(jnp.sum(lam_q2.astype(f32) * lam_k2.astype(f32))) + lambda_init)
    slopes = alibi_slopes(H)
    kpos = jnp.arange(T)
    n_blocks = T // Q_BLOCK
    q_blocks = jnp.moveaxis(q.reshape(B, n_blocks, Q_BLOCK, H, 2, d), 1, 0)

    def attend_block(args):
        q_blk, blk = args
        qpos = blk * Q_BLOCK + jnp.arange(Q_BLOCK)
        dist = (qpos[:, None] - kpos[None, :]).astype(f32)
        bias = -slopes[:, None, None] * dist
        s = jnp.einsum('bqhcd,bkhcd->bhcqk', q_blk, k) + bias[None, :, None]
        s = jnp.where(dist >= 0, s, -jnp.inf)
        p = jax.nn.softmax(s, axis=-1)
        attn = p[:, :, 0] - lam * p[:, :, 1]
        return jnp.einsum('bhqk,bkhe->bqhe', attn, v)

    o = lax.map(attend_block, (q_blocks, jnp.arange(n_blocks)))
    o = jnp.moveaxis(o, 0, 1).reshape(B, T, H, dv)
    o = rmsnorm(o, sub_g) * (1.0 - lambda_init)
    return o.reshape(B, T, DIFF_V_WIDTH) @ w_out


def grouped_moe(h, router_w, router_bias, w_gate, w_up, w_down):
    B, T, D = h.shape
    f32 = jnp.float32
    xt = h.reshape(B * T, D)
    per_group = N_EXPERTS // N_GROUPS
    scores = jax.nn.sigmoid((xt @ router_w).astype(f32))
    sel = scores + router_bias.astype(f32)
    group_score = lax.top_k(sel.reshape(-1, N_GROUPS, per_group), TOP_K)[0].sum(-1)
    best_group = jnp.argmax(group_score, axis=-1)
    in_group = (jnp.arange(N_EXPERTS) // per_group)[None, :] == best_group[:, None]
    _, idx = lax.top_k(jnp.where(in_group, sel, -jnp.inf), TOP_K)
    wts = jnp.take_along_axis(scores, idx, axis=-1)
    wts = wts / jnp.sum(wts, axis=-1, keepdims=True)
    combine = jnp.sum(jax.nn.one_hot(idx, N_EXPERTS, dtype=f32) * wts[..., None], axis=1)
    y = jnp.zeros((B * T, D), f32)
    for e in range(N_EXPERTS):
        hid = jax.nn.silu(xt @ w_gate[e]) * (xt @ w_up[e])
        y = y + combine[:, e:e + 1] * (hid @ w_down[e])
    return y.reshape(B, T, D).astype(h.dtype)


def setup_inputs(seed: int = 0) -> dict:
    key = jax.random.key(seed)
    ks = jax.random.split(key, 31)
    L = DEPTH
    f32 = jnp.float32

    def nrm(i, shape, scale):
        return jax.random.normal(ks[i], shape, f32) * scale

    def gain(i, shape):
        return 1.0 + nrm(i, shape, 0.05)

    return {
        'x': nrm(0, (BATCH, SEQ, D_MODEL), 1.0),
        'c': nrm(1, (BATCH, D_MODEL), 1.0),
        'ada_w': nrm(2, (L, D_MODEL, ADA_CHUNKS * D_MODEL), 0.5 * D_MODEL ** -0.5),
        'ada_b': nrm(3, (L, ADA_CHUNKS * D_MODEL), 0.02),
        'norm1_g': gain(4, (L, D_MODEL)),
        'w_in': nrm(5, (L, D_MODEL, IN_COLS), D_MODEL ** -0.5),
        'gdn_conv_w': nrm(6, (L, GDN_CONV_WIDTH, 3 * GDN_WIDTH), GDN_CONV_WIDTH ** -0.5),
        'gdn_a_log': jnp.log(jax.random.uniform(ks[7], (L, GDN_HEADS), f32, 1.0, 16.0)),
        'gdn_dt_bias': nrm(8, (L, GDN_HEADS), 0.1),
        'gdn_norm_g': gain(9, (L, GDN_HEAD_DIM)),
        'gdn_w_out': nrm(10, (L, GDN_WIDTH, D_MODEL), GDN_WIDTH ** -0.5),
        'conf_dw_w': nrm(11, (L, CONF_KERNEL, CONF_CHANNELS), CONF_KERNEL ** -0.5),
        'conf_dw_b': nrm(12, (L, CONF_CHANNELS), 0.02),
        'conf_ln_g': gain(13, (L, CONF_CHANNELS)),
        'conf_ln_b': nrm(14, (L, CONF_CHANNELS), 0.02),
        'conf_w_out': nrm(15, (L, CONF_CHANNELS, D_MODEL), CONF_CHANNELS ** -0.5),
        'diff_q_norm_g': gain(16, (L, DIFF_QK_DIM)),
        'diff_k_norm_g': gain(17, (L, DIFF_QK_DIM)),
        'diff_lambda_q1': nrm(18, (L, DIFF_QK_DIM), 0.1),
        'diff_lambda_k1': nrm(19, (L, DIFF_QK_DIM), 0.1),
        'diff_lambda_q2': nrm(20, (L, DIFF_QK_DIM), 0.1),
        'diff_lambda_k2': nrm(21, (L, DIFF_QK_DIM), 0.1),
        'diff_sub_g': gain(22, (L, DIFF_V_DIM)),
        'diff_w_out': nrm(23, (L, DIFF_V_WIDTH, D_MODEL), DIFF_V_WIDTH ** -0.5),
        'w_o': nrm(24, (L, D_MODEL, D_MODEL), D_MODEL ** -0.5),
        'norm2_g': gain(25, (L, D_MODEL)),
        'router_w': nrm(26, (D_MODEL, N_EXPERTS), D_MODEL ** -0.5),
        'router_bias': nrm(27, (N_EXPERTS,), 0.01),
        'exp_w_gate': nrm(28, (L, N_EXPERTS, D_MODEL, EXPERT_FF), D_MODEL ** -0.5),
        'exp_w_up': nrm(29, (L, N_EXPERTS, D_MODEL, EXPERT_FF), D_MODEL ** -0.5),
        'exp_w_down': nrm(30, (L, N_EXPERTS, EXPERT_FF, D_MODEL), EXPERT_FF ** -0.5),
    }


def reference(x, c, ada_w, ada_b, norm1_g, w_in, gdn_conv_w, gdn_a_log, gdn_dt_bias,
              gdn_norm_g, gdn_w_out, conf_dw_w, conf_dw_b, conf_ln_g, conf_ln_b, conf_w_out,
              diff_q_norm_g, diff_k_norm_g, diff_lambda_q1, diff_lambda_k1, diff_lambda_q2,
              diff_lambda_k2, diff_sub_g, diff_w_out, w_o, norm2_g, router_w, router_bias,
              exp_w_gate, exp_w_up, exp_w_down):
    cond = jax.nn.silu(c)
    for l in range(DEPTH):
        mod = cond @ ada_w[l] + ada_b[l]
        shift1, scale1, gate1, shift2, scale2, gate2 = [m[:, None, :] for m in jnp.split(mod, ADA_CHUNKS, axis=-1)]
        h = rmsnorm(x, norm1_g[l]) * (1.0 + scale1) + shift1
        (g_q, g_k, g_v, g_z, g_b, g_a, conf_in, d_q, d_k, d_v,
         gate_a, gate_b, gate_c) = split_columns(h @ w_in[l])
        y_a = gdn_mixer(g_q, g_k, g_v, g_z, g_b, g_a, gdn_conv_w[l], gdn_a_log[l], gdn_dt_bias[l],
                        gdn_norm_g[l], gdn_w_out[l])
        y_b = conformer_conv_mixer(conf_in, conf_dw_w[l], conf_dw_b[l], conf_ln_g[l], conf_ln_b[l],
                                   conf_w_out[l])
        lambda_init = 0.8 - 0.6 * math.exp(-0.3 * l)
        y_c = diff_attention_mixer(d_q, d_k, d_v, diff_q_norm_g[l], diff_k_norm_g[l],
                                   diff_lambda_q1[l], diff_lambda_k1[l], diff_lambda_q2[l],
                                   diff_lambda_k2[l], diff_sub_g[l], diff_w_out[l], lambda_init)
        mixed = (jax.nn.sigmoid(gate_a) * y_a + jax.nn.sigmoid(gate_b) * y_b
                 + jax.nn.sigmoid(gate_c) * y_c)
        x = x + gate1 * (mixed @ w_o[l])
        h2 = rmsnorm(x, norm2_g[l]) * (1.0 + scale2) + shift2
        x = x + gate2 * grouped_moe(h2, router_w, router_bias, exp_w_gate[l], exp_w_up[l], exp_w_down[l])
    return x
```

```python
from contextlib import ExitStack
import numpy as np
import concourse.bass as bass
import concourse.mybir as mybir
from concourse.bass_utils import run_bass_kernel_spmd

F32 = mybir.dt.float32
BF16 = mybir.dt.bfloat16
AF = mybir.ActivationFunctionType
ALU = mybir.AluOpType
AX = mybir.AxisListType

D = 4096
KT = D // 128
NE = 16
FF = 1024
NCORES = 8
BIG = 1.0e4


class Buf:
    __slots__ = ("name", "w", "r")

    def __init__(self, name):
        self.name = name
        self.w = None
        self.r = []


class Prog:
    NQ = 6

    def __init__(self):
        nc = bass.Bass("TRN2", target_bir_lowering=False)
        self.nc = nc
        self.eng = {"pe": nc.tensor, "act": nc.scalar, "dve": nc.vector, "pool": nc.gpsimd, "sp": nc.sync}
        self.sem = {e: nc.alloc_semaphore("s_" + e) for e in self.eng}
        self.cnt = {e: 0 for e in self.eng}
        self.dsem = {q: [nc.alloc_semaphore("d_%s%d" % (q, i)) for i in range(self.NQ)] for q in ("sp", "pool", "act")}
        self.dcnt = {q: 0 for q in self.dsem}
        self.csem = nc.alloc_semaphore("cc")
        self.ccnt = 0
        self.waited = {}
        self.dlast = {}
        self.phase = None
        self.nalloc = 0
        self.nbuf = 0

    def buf(self, name=None):
        self.nbuf += 1
        return Buf(name or "b%d" % self.nbuf)

    def sbuf(self, name, shape, dt):
        if self.phase is None:
            return self.nc.alloc_sbuf_tensor(name, list(shape), dt)
        self.nalloc += 1
        return self.phase.enter_context(self.nc.sbuf_tensor("%s_%d" % (name, self.nalloc), list(shape), dt))

    def psum(self, name, shape, dt=F32):
        if self.phase is None:
            return self.nc.alloc_psum_tensor(name, list(shape), dt)
        self.nalloc += 1
        return self.phase.enter_context(self.nc.psum_tensor("%s_%d" % (name, self.nalloc), list(shape), dt))

    def begin_phase(self):
        self.barrier()
        if self.phase is not None:
            self.phase.close()
        self.phase = ExitStack()

    def dramu(self, name, shape, dt):
        self.nalloc += 1
        return self.nc.dram_tensor("%s_%d" % (name, self.nalloc), list(shape), dt, kind="Internal")

    def dram(self, name, shape, dt, kind="Internal"):
        return self.nc.dram_tensor(name, list(shape), dt, kind=kind)

    def _wait(self, e, key, sem, val):
        if val <= 0:
            return
        k = (e, key)
        if self.waited.get(k, 0) >= val:
            return
        self.eng[e].wait_ge(sem, val)
        self.waited[k] = val

    def _deps(self, e, reads, writes):
        toks = []
        for b in reads:
            if b.w is not None:
                toks.append(b.w)
        for b in writes:
            if b.w is not None:
                toks.append(b.w)
            toks.extend(b.r)
        for (key, sem, val) in toks:
            if key == "pe" and e == "pe":
                continue
            self._wait(e, key, sem, val)

    def _register(self, tok, reads, writes):
        for b in reads:
            b.r.append(tok)
            if len(b.r) > 48:
                best = {}
                for t in b.r:
                    if t[0] not in best or best[t[0]][2] < t[2]:
                        best[t[0]] = t
                b.r = list(best.values())
        for b in writes:
            b.w = tok
            b.r = []

    def op(self, e, fn, reads=(), writes=()):
        self._deps(e, reads, writes)
        ins = fn(self.eng[e])
        self.cnt[e] += 1
        ins.then_inc(self.sem[e], 1)
        tok = (e, self.sem[e], self.cnt[e])
        self._register(tok, reads, writes)
        return tok

    def dma(self, q, out, in_, reads=(), writes=(), **kw):
        j = self.dcnt[q]
        slot = j % self.NQ
        sem = self.dsem[q][slot]
        val = 16 * (j // self.NQ + 1)
        key = ("d", q, slot)
        self._wait(q, key, sem, val - 16)
        self._deps(q, reads, writes)
        ins = self.eng[q].dma_start(out=out, in_=in_, **kw)
        ins.then_inc(sem, 16)
        self.dcnt[q] += 1
        self.dlast[key] = (sem, val)
        tok = (key, sem, val)
        self._register(tok, reads, writes)
        return tok

    def dma_gather(self, out, in_rows, idx_ap, nrows, reads=(), writes=()):
        q = "pool"
        j = self.dcnt[q]
        slot = j % self.NQ
        sem = self.dsem[q][slot]
        val = 16 * (j // self.NQ + 1)
        key = ("d", q, slot)
        self._wait(q, key, sem, val - 16)
        self._deps(q, reads, writes)
        ins = self.nc.gpsimd.indirect_dma_start(
            out=out, out_offset=None, in_=in_rows, in_offset=bass.IndirectOffsetOnAxis(ap=idx_ap, axis=0),
            bounds_check=nrows - 1, oob_is_err=False)
        ins.then_inc(sem, 16)
        self.dcnt[q] += 1
        self.dlast[key] = (sem, val)
        tok = (key, sem, val)
        self._register(tok, reads, writes)
        return tok

    def barrier(self):
        for e in self.eng:
            for e2 in self.eng:
                if e2 != e:
                    self._wait(e, e2, self.sem[e2], self.cnt[e2])
            for key, (sem, val) in self.dlast.items():
                self._wait(e, key, sem, val)
            self._wait(e, "cc", self.csem, self.ccnt)

    def dma_k(self, q, out, in_, nk, parts, reads=(), writes=()):
        step = nk // parts
        for i in range(parts):
            self.dma(q, out[:, i * step:(i + 1) * step], in_[:, i * step:(i + 1) * step], reads=reads, writes=writes)

    def allgather(self, in_ap, out_ap, reads, writes):
        self._deps("pool", reads, writes)
        ins = self.nc.gpsimd.collective_compute(
            "AllGather", ALU.bypass, replica_groups=[list(range(NCORES))], ins=[in_ap], outs=[out_ap])
        self.ccnt += 1
        ins.then_inc(self.csem, 1)
        tok = ("cc", self.csem, self.ccnt)
        self._register(tok, reads, writes)
        self._wait("pool", "cc", self.csem, self.ccnt)
        return tok

    def finish(self, bufs):
        for b in bufs:
            if b.w is not None:
                key, sem, val = b.w
                self._wait("sp", key, sem, val)


def gather_weight(p, name, src, rows, cols, rpc):
    nch = rows // rpc
    assert nch * rpc == rows
    g = p.dram(name + "_g", [nch, NCORES, rpc, cols], BF16)
    stage = p.dram(name + "_st", [2, rpc, cols], BF16)
    sb = [p.buf(), p.buf()]
    gb = p.buf(name + "_gb")
    src_b = p.buf()
    for c in range(nch):
        s = c % 2
        p.dma("pool", stage.ap()[s], src.ap()[c * rpc:(c + 1) * rpc, :], reads=[src_b], writes=[sb[s]])
        p.allgather(stage.ap()[s], g.ap()[c].rearrange("r j f -> (r j) f"), reads=[sb[s]], writes=[gb])
    return g, gb


def gather_nat(p, name, src, rows, cols, rpc):
    g, gb = gather_weight(p, name, src, rows, cols, rpc)
    nat = p.dram(name + "_n", [NCORES * rows, cols], BF16)
    nb = p.buf(name + "_nb")
    for r in range(NCORES):
        p.dma("sp", nat.ap()[r * rows:(r + 1) * rows, :].rearrange("(c j) n -> c j n", j=rpc), g.ap()[:, r, :, :],
              reads=[gb], writes=[nb])
    return nat, nb


def build(L, NT, do_moe=True, stop_after=None):
    TC = 256
    NCH = NT // TC
    p = Prog()
    nc = p.nc
    xT = nc.dram_tensor("xT", [D, NT], F32, kind="ExternalInput")
    cT = nc.dram_tensor("cT", [D, 2], F32, kind="ExternalInput")
    bsel = nc.dram_tensor("bsel", [128, 2], F32, kind="ExternalInput")
    ident = nc.dram_tensor("ident", [128, 128], F32, kind="ExternalInput")
    ada_w = nc.dram_tensor("ada_w", [L, D, 3072], F32, kind="ExternalInput")
    ada_b = nc.dram_tensor("ada_b", [L, 128, 24], F32, kind="ExternalInput")
    norm2_g = nc.dram_tensor("norm2_g", [L, 128, KT], F32, kind="ExternalInput")
    router_w = nc.dram_tensor("router_w", [D, NE], F32, kind="ExternalInput")
    router_b = nc.dram_tensor("router_b", [128, NE], F32, kind="ExternalInput")
    wg = [nc.dram_tensor("wg%d" % l, [8192, FF], F32, kind="ExternalInput") for l in range(L)]
    wu = [nc.dram_tensor("wu%d" % l, [8192, FF], F32, kind="ExternalInput") for l in range(L)]
    wd = [nc.dram_tensor("wd%d" % l, [2048, D], F32, kind="ExternalInput") for l in range(L)]
    yT = nc.dram_tensor("yT", [D, NT], F32, kind="ExternalOutput")
    xmid = [p.dram("xmid%d" % l, [D, NT], F32) for l in range(L - 1)]

    ones_f = p.sbuf("ones_f", [128, 128], F32)
    ident_sb = p.sbuf("ident_sb", [128, 128], F32)
    bsel_sb = p.sbuf("bsel_sb", [128, 2], F32)
    rb_sb = p.sbuf("rb_sb", [128, NE], F32)
    rw_sb = p.sbuf("rw_sb", [128, KT, NE], F32)
    cT_sb = p.sbuf("cT_sb", [128, KT, 2], F32)
    cond_bf = p.sbuf("cond_bf", [128, KT, 2], BF16)
    b_const = p.buf("const")
    ext = p.buf("ext")
    p.op("dve", lambda e: e.memset(ones_f[:], 1.0), writes=[b_const])
    p.dma("sp", ident_sb[:], ident.ap(), reads=[ext], writes=[b_const])
    p.dma("sp", bsel_sb[:], bsel.ap(), reads=[ext], writes=[b_const])
    p.dma("sp", rb_sb[:], router_b.ap(), reads=[ext], writes=[b_const])
    p.dma("sp", rw_sb[:], router_w.ap().rearrange("(k p) e -> p k e", p=128), reads=[ext], writes=[b_const])
    p.dma("sp", cT_sb[:], cT.ap().rearrange("(k p) b -> p k b", p=128), reads=[ext], writes=[b_const])
    p.op("act", lambda e: e.activation(out=cond_bf[:], in_=cT_sb[:], func=AF.Silu), reads=[b_const], writes=[b_const])

    wbuf = [p.sbuf("wbuf%d" % i, [128, 16384], BF16) for i in range(2)]
    wbb = [p.buf("wbuf0"), p.buf("wbuf1")]
    ps_small = p.psum("ps_small", [128, 512], F32)
    pss_b = p.buf("ps_small")
    mod_loc = [p.dram("mod_loc%d" % l, [128, 48], F32) for l in range(L)]
    mod_full = [p.dram("mod_full%d" % l, [NCORES * 128, 48], F32) for l in range(L)]
    modT_sb = p.sbuf("modT_sb", [128, 24, 2], F32)
    adab_sb = p.sbuf("adab_sb", [128, 24], F32)
    modT_b = p.buf("modT")
    mod_full_b = [p.buf() for l in range(L)]
    nload = 0
    for l in range(L):
        p.dma("sp", adab_sb[:], ada_b.ap()[l], reads=[ext], writes=[modT_b])
        for cg in range(6):
            s = nload % 2
            nload += 1
            wv = wbuf[s][:].rearrange("p (k n) -> p k n", k=KT)
            p.dma_k("pool", wv, ada_w.ap()[l].rearrange("(k p) n -> p k n", p=128)[:, :, cg * 512:(cg + 1) * 512], KT, 4,
                    reads=[ext], writes=[wbb[s]])
            for ct in range(4):
                col = cg * 4 + ct
                for k in range(KT):
                    p.op("pe", lambda e, k=k, ct=ct, wv=wv: e.matmul(
                        ps_small[:, 0:2], wv[:, k, ct * 128:(ct + 1) * 128], cond_bf[:, k, :],
                        start=(k == 0), stop=(k == KT - 1)),
                        reads=[wbb[s], b_const], writes=[pss_b])
                p.op("dve", lambda e, col=col: e.tensor_scalar(
                    out=modT_sb[:, col, :], in0=ps_small[:, 0:2], scalar1=adab_sb[:, col:col + 1], scalar2=None,
                    op0=ALU.add), reads=[pss_b, modT_b], writes=[modT_b])
        mlb = p.buf()
        p.dma("sp", mod_loc[l].ap(), modT_sb[:].rearrange("p c b -> p (c b)"), reads=[modT_b], writes=[mlb])
        p.allgather(mod_loc[l].ap(), mod_full[l].ap(), reads=[mlb], writes=[mod_full_b[l]])

    if not do_moe:
        L_moe = 0
    gw = []
    for l in range(L):
        g1, b1 = gather_weight(p, "wg%d" % l, wg[l], 8192, FF, 128)
        g2, b2 = gather_weight(p, "wu%d" % l, wu[l], 8192, FF, 128)
        g3, b3 = gather_weight(p, "wd%d" % l, wd[l], 2048, D, 32)
        gw.append((g1, b1, g2, b2, g3, b3))

    TB = NT * 4
    NTOT = NT * NCORES
    TG = min(NT, 1024)
    nth = NT // TG
    TCH = min(512, TG)
    NQT = TB // 128
    norm1_g = nc.dram_tensor("norm1_g", [L, 128, KT], F32, kind="ExternalInput")
    w_mix = [nc.dram_tensor("w_mix%d" % l, [D, 1280], F32, kind="ExternalInput") for l in range(L)]
    w_gat = [nc.dram_tensor("w_gat%d" % l, [512, 3 * D], F32, kind="ExternalInput") for l in range(L)]
    w_oa = [[nc.dram_tensor("w_o%s%d" % (m, l), [128, D], F32, kind="ExternalInput") for m in "abc"] for l in range(L)]
    w_o = [nc.dram_tensor("w_oo%d" % l, [512, D], F32, kind="ExternalInput") for l in range(L)]
    smallp = {}
    for nm, shp in (("gconv", [128, 12]), ("gscal", [128, 2]), ("gng", [128, 128]), ("cdw", [128, 31]), ("cdb", [128, 1]),
                    ("clng", [128, 8]), ("clnb", [128, 8]), ("dqg", [128, 1]), ("dkg", [128, 1]), ("dlam", [128, 256]),
                    ("dsg", [128, 128])):
        smallp[nm] = nc.dram_tensor(nm, [L] + shp, F32, kind="ExternalInput")
    abias_d = nc.dram_tensor("abias", [128, NQT], F32, kind="ExternalInput")
    cmask = {nm: nc.dram_tensor(nm, [128, 128], F32, kind="ExternalInput") for nm in ("triu", "trils", "blk")}
    sel63_d = nc.dram_tensor("sel63", [64, 128], F32, kind="ExternalInput")
    NCHF = NT // TC
    gidx_d = nc.dram_tensor("gidx", [128, NCHF * 8], mybir.dt.int32, kind="ExternalInput")
    triu = p.sbuf("triu_sb", [128, 128], F32)
    trils = p.sbuf("trils_sb", [128, 128], F32)
    blk = p.sbuf("blk_sb", [128, 128], F32)
    sel63 = p.sbuf("sel63_sb", [64, 128], F32)
    gidx = p.sbuf("gidx_sb", [128, NCHF * 8], mybir.dt.int32)
    rsel_d = nc.dram_tensor("rsel", [128, NCORES], F32, kind="ExternalInput")
    rsel_sb = p.sbuf("rsel_sb", [128, NCORES], F32)
    p.dma("sp", rsel_sb[:], rsel_d.ap(), reads=[ext], writes=[b_const])
    p.dma("sp", triu[:], cmask["triu"].ap(), reads=[ext], writes=[b_const])
    p.dma("sp", trils[:], cmask["trils"].ap(), reads=[ext], writes=[b_const])
    p.dma("sp", blk[:], cmask["blk"].ap(), reads=[ext], writes=[b_const])
    p.dma("sp", sel63[:], sel63_d.ap(), reads=[ext], writes=[b_const])
    p.dma("sp", gidx[:], gidx_d.ap(), reads=[ext], writes=[b_const])
    gwm = []
    for l in range(L):
        gg = gather_nat(p, "wgat%d" % l, w_gat[l], 512, 3 * D, 8)
        ga = [gather_nat(p, "wo%s%d" % (m, l), w_oa[l][i], 128, D, 32) for i, m in enumerate("abc")]
        go = gather_nat(p, "woo%d" % l, w_o[l], 512, D, 32)
        gwm.append((gg, ga, go))

    def rstd_from(t_ap, bt, scale, eps):
        p.op("dve", lambda e: e.tensor_scalar(out=t_ap, in0=t_ap, scalar1=scale, scalar2=eps, op0=ALU.mult, op1=ALU.add),
             reads=[bt], writes=[bt])
        p.op("act", lambda e: e.activation(out=t_ap, in_=t_ap, func=AF.Ln), reads=[bt], writes=[bt])
        p.op("act", lambda e: e.activation(out=t_ap, in_=t_ap, func=AF.Exp, scale=-0.5), reads=[bt], writes=[bt])

    def load_mod3(l, c0, ng_dram):
        modc = p.sbuf("modc", [128, 3, KT, 2], F32)
        A_ = p.sbuf("A_", [128, KT], F32)
        B_ = p.sbuf("B_", [128, KT], F32)
        G_ = p.sbuf("G_", [128, KT], F32)
        ng = p.sbuf("ng", [128, KT], F32)
        tk = p.sbuf("tk", [128, KT], F32)
        bm = p.buf("modx")
        mf = mod_full[l].ap().rearrange("(r p) (c b) -> p r c b", p=128, b=2)
        for ci in range(3):
            t = (c0 + ci) * 32
            tend = t + 32
            while t < tend:
                r_, c_ = t // 24, t % 24
                n = min(24 - c_, tend - t)
                k0 = t - (c0 + ci) * 32
                p.dma("sp", modc[:, ci, k0:k0 + n, :], mf[:, r_, c_:c_ + n, :], reads=[mod_full_b[l]], writes=[bm])
                t += n
        p.dma("sp", ng[:], ng_dram.ap()[l], reads=[ext], writes=[bm])
        for ci, out_t in ((0, B_), (1, A_), (2, G_)):
            p.op("dve", lambda e: e.tensor_scalar(out=tk[:], in0=modc[:, ci, :, 1], scalar1=bsel_sb[:, 1:2],
                                                  scalar2=None, op0=ALU.mult), reads=[bm, b_const], writes=[bm])
            p.op("dve", lambda e: e.scalar_tensor_tensor(out=out_t[:], in0=modc[:, ci, :, 0], scalar=bsel_sb[:, 0:1],
                                                         in1=tk[:], op0=ALU.mult, op1=ALU.add), reads=[bm, b_const], writes=[bm])
        p.op("dve", lambda e: e.scalar_tensor_tensor(out=A_[:], in0=A_[:], scalar=1.0, in1=ng[:],
                                                     op0=ALU.add, op1=ALU.mult), reads=[bm], writes=[bm])
        return A_, B_, G_, bm

    def transpose_to(ps_ap, in_ap, K, bufs_r, buf_w):
        p.op("pe", lambda e: e.matmul(ps_ap, in_ap, ident_sb[0:K, 0:K], start=True, stop=True),
             reads=list(bufs_r) + [b_const], writes=[buf_w])

    def mix_layer(l, src, src_b):
        nonlocal nload
        lam_init = 0.8 - 0.6 * float(np.exp(-0.3 * l))
        (gg, ggb), ga, (go, gob) = gwm[l]
        hloc = p.dramu("hloc", [nth, D, TG], BF16)
        Gh = p.dramu("Gh", [KT * nth, NCORES, 128, TG], BF16)
        PT = p.dramu("PT", [1280, NTOT], F32)
        oml = [p.dramu("oml%d" % m, [NTOT // 512, 128, 512], F32) for m in range(3)]
        Gm = [p.dramu("Gm%d" % m, [NTOT // 512, NCORES, 128, 512], F32) for m in range(3)]
        x1 = p.dramu("x1", [D, NT], F32)
        b_hloc, b_Gh, b_PT, b_x1 = p.buf("hloc"), p.buf("Gh"), p.buf("PT"), p.buf("x1")
        b_oml = [p.buf() for _ in range(3)]
        b_Gm = [p.buf() for _ in range(3)]

        p.begin_phase()
        A1, B1, G1, bm1 = load_mod3(l, 0, norm1_g)
        x_sb = p.sbuf("xa", [128, KT, TC], F32)
        hb = p.sbuf("hb", [128, KT, TC], BF16)
        sq = p.sbuf("sqa", [128, TC], F32)
        rs = p.sbuf("rsa", [128, TC], F32)
        ps_ss = p.psum("ps_ssa", [128, 512], F32)
        bx, bhb, bsq, brs, bps = p.buf(), p.buf(), p.buf(), p.buf(), p.buf()
        for ch in range(NT // TC):
            t0 = ch * TC
            p.dma_k("sp", x_sb[:], src.ap().rearrange("(k p) n -> p k n", p=128)[:, :, t0:t0 + TC], KT, 4, reads=[src_b], writes=[bx])
            for k in range(KT):
                p.op("act", lambda e, k=k: e.activation(out=sq[:], in_=x_sb[:, k, :], func=AF.Square), reads=[bx], writes=[bsq])
                p.op("pe", lambda e, k=k: e.matmul(ps_ss[:, 0:TC], ones_f[:], sq[:], start=(k == 0), stop=(k == KT - 1)),
                     reads=[bsq, b_const], writes=[bps])
            p.op("dve", lambda e: e.tensor_copy(out=rs[:], in_=ps_ss[:, 0:TC]), reads=[bps], writes=[brs])
            rstd_from(rs[:], brs, 1.0 / D, 1e-6)
            for k in range(KT):
                p.op("dve", lambda e, k=k: e.tensor_tensor(out=x_sb[:, k, :], in0=x_sb[:, k, :], in1=rs[:], op=ALU.mult),
                     reads=[bx, brs], writes=[bx])
                p.op("dve", lambda e, k=k: e.tensor_scalar(out=hb[:, k, :], in0=x_sb[:, k, :], scalar1=A1[:, k:k + 1],
                                                           scalar2=B1[:, k:k + 1], op0=ALU.mult, op1=ALU.add),
                     reads=[bx, bm1], writes=[bhb])
            p.dma_k("sp", hloc.ap()[t0 // TG].rearrange("(k p) n -> p k n", p=128)[:, :, t0 % TG:t0 % TG + TC], hb[:], KT, 4, reads=[bhb], writes=[b_hloc])
        for k in range(KT):
            for th in range(nth):
                p.allgather(hloc.ap()[th, k * 128:(k + 1) * 128, :],
                            Gh.ap()[k * nth + th].rearrange("r j f -> (r j) f"), reads=[b_hloc], writes=[b_Gh])

        if stop_after == 'A':
            return src, src_b
        p.begin_phase()
        wm = p.sbuf("wm", [128, KT, 1280], BF16)
        bwm = p.buf()
        p.dma_k("pool", wm[:], w_mix[l].ap().rearrange("(k p) n -> p k n", p=128), KT, 8, reads=[ext], writes=[bwm])
        ob = [p.sbuf("ob%d" % i, [128, TCH], F32) for i in range(2)]
        bob = [p.buf(), p.buf()]
        psb = [p.psum("psb%d" % i, [128, 512], F32) for i in range(2)]
        bpsb = [p.buf(), p.buf()]
        it = 0
        for r in range(NCORES):
            for th in range(nth):
                for off in range(0, TG, TCH):
                    s = nload % 2
                    nload += 1
                    hv = wbuf[s][:, 0:KT * TCH].rearrange("p (k n) -> p k n", k=KT)
                    p.dma_k("sp", hv, Gh.ap()[th::nth, r, :, off:off + TCH].rearrange("k p n -> p k n"), KT, 4,
                            reads=[b_Gh], writes=[wbb[s]])
                    g0 = r * NT + th * TG + off
                    for ct in range(10):
                        i2 = it % 2
                        it += 1
                        for k in range(KT):
                            p.op("pe", lambda e, k=k, ct=ct, i2=i2, hv=hv: e.matmul(
                                psb[i2][:, 0:TCH], wm[:, k, ct * 128:(ct + 1) * 128], hv[:, k, :],
                                start=(k == 0), stop=(k == KT - 1)), reads=[bwm, wbb[s]], writes=[bpsb[i2]])
                        eng = "act" if i2 == 0 else "dve"
                        if eng == "act":
                            p.op("act", lambda e, i2=i2: e.activation(out=ob[i2][:], in_=psb[i2][:, 0:TCH], func=AF.Copy),
                                 reads=[bpsb[i2]], writes=[bob[i2]])
                        else:
                            p.op("dve", lambda e, i2=i2: e.tensor_copy(out=ob[i2][:], in_=psb[i2][:, 0:TCH]),
                                 reads=[bpsb[i2]], writes=[bob[i2]])
                        p.dma("sp", PT.ap()[ct * 128:(ct + 1) * 128, g0:g0 + TCH], ob[i2][:], reads=[bob[i2]], writes=[b_PT])

        if stop_after == 'B':
            return src, src_b
        p.begin_phase()
        CB = min(2048, TB)
        cw = p.sbuf("cw", [128, 31], F32)
        cbias = p.sbuf("cbias", [128, 1], F32)
        va = p.sbuf("va", [128, 30 + CB], F32)
        gt = p.sbuf("gt", [128, 30 + CB], F32)
        acc = p.sbuf("acc", [128, CB], F32)
        bcw, bva, bgt, bacc = p.buf(), p.buf(), p.buf(), p.buf()
        p.dma("sp", cw[:], smallp["cdw"].ap()[l], reads=[ext], writes=[bcw])
        p.dma("sp", cbias[:], smallp["cdb"].ap()[l], reads=[ext], writes=[bcw])
        for b in range(2):
            for blk_i in range(TB // CB):
                g0 = b * TB + blk_i * CB
                if blk_i == 0:
                    p.op("dve", lambda e: e.memset(va[:, 0:30], 0.0), writes=[bva])
                    p.op("dve", lambda e: e.memset(gt[:, 0:30], 0.0), writes=[bgt])
                    p.dma("sp", va[:, 30:30 + CB], PT.ap()[512:640, g0:g0 + CB], reads=[b_PT], writes=[bva])
                    p.dma("sp", gt[:, 30:30 + CB], PT.ap()[640:768, g0:g0 + CB], reads=[b_PT], writes=[bgt])
                else:
                    p.dma("sp", va[:], PT.ap()[512:640, g0 - 30:g0 + CB], reads=[b_PT], writes=[bva])
                    p.dma("sp", gt[:], PT.ap()[640:768, g0 - 30:g0 + CB], reads=[b_PT], writes=[bgt])
                p.op("act", lambda e: e.activation(out=gt[:], in_=gt[:], func=AF.Sigmoid), reads=[bgt], writes=[bgt])
                p.op("dve", lambda e: e.tensor_tensor(out=va[:], in0=va[:], in1=gt[:], op=ALU.mult), reads=[bva, bgt], writes=[bva])
                p.op("dve", lambda e: e.tensor_scalar(out=acc[:], in0=va[:, 30:30 + CB], scalar1=cw[:, 30:31], scalar2=cbias[:, 0:1],
                                                      op0=ALU.mult, op1=ALU.add), reads=[bva, bcw], writes=[bacc])
                for j in range(30):
                    p.op("dve", lambda e, j=j: e.scalar_tensor_tensor(out=acc[:], in0=va[:, j:j + CB], scalar=cw[:, j:j + 1], in1=acc[:],
                                                                     op0=ALU.mult, op1=ALU.add), reads=[bva, bcw, bacc], writes=[bacc])
                p.dma("sp", oml[1].ap()[g0 // 512:(g0 + CB) // 512].rearrange("c p n -> p c n"), acc[:].rearrange("p (c n) -> p c n", n=512),
                      reads=[bacc], writes=[b_oml[1]])

        if stop_after == 'C':
            return src, src_b
        p.begin_phase()
        qn = p.sbuf("qn", [128, TB], BF16)
        kn = p.sbuf("kn", [128, TB], BF16)
        V1 = p.sbuf("V1", [128, NQT, 132], BF16)
        oT = p.sbuf("oT", [128, TB], F32)
        bqn, bkn, bV1, boT = p.buf(), p.buf(), p.buf(), p.buf()
        ld = p.sbuf("ld", [128, 512], F32)
        sqd = p.sbuf("sqd", [128, 512], F32)
        rsd = p.sbuf("rsd", [128, 512], F32)
        bld, bsqd, brsd = p.buf(), p.buf(), p.buf()
        qg = p.sbuf("qg", [128, 1], F32)
        kg = p.sbuf("kg", [128, 1], F32)
        lamt = p.sbuf("lamt", [128, 4, 64], F32)
        lam2 = p.sbuf("lam2", [128, 2], F32)
        nlam = p.sbuf("nlam", [128, 1], F32)
        sgt = p.sbuf("sgt", [128, 128], F32)
        abias = p.sbuf("abias_sb", [128, NQT], F32)
        bpar = p.buf()
        p.dma("sp", qg[:], smallp["dqg"].ap()[l], reads=[ext], writes=[bpar])
        p.dma("sp", kg[:], smallp["dkg"].ap()[l], reads=[ext], writes=[bpar])
        p.dma("sp", lamt[:].rearrange("p a b -> p (a b)"), smallp["dlam"].ap()[l], reads=[ext], writes=[bpar])
        p.dma("sp", sgt[:], smallp["dsg"].ap()[l], reads=[ext], writes=[bpar])
        p.dma("sp", abias[:], abias_d.ap(), reads=[ext], writes=[bpar])
        P_ = [bpar]
        p.op("dve", lambda e: e.tensor_tensor(out=lamt[:, 0, :], in0=lamt[:, 0, :], in1=lamt[:, 1, :], op=ALU.mult), reads=P_, writes=P_)
        p.op("dve", lambda e: e.tensor_tensor(out=lamt[:, 2, :], in0=lamt[:, 2, :], in1=lamt[:, 3, :], op=ALU.mult), reads=P_, writes=P_)
        p.op("dve", lambda e: e.tensor_reduce(out=lam2[:, 0:1], in_=lamt[:, 0, :], axis=AX.X, op=ALU.add), reads=P_, writes=P_)
        p.op("dve", lambda e: e.tensor_reduce(out=lam2[:, 1:2], in_=lamt[:, 2, :], axis=AX.X, op=ALU.add), reads=P_, writes=P_)
        p.op("act", lambda e: e.activation(out=lam2[:], in_=lam2[:], func=AF.Exp), reads=P_, writes=P_)
        p.op("dve", lambda e: e.tensor_tensor(out=nlam[:], in0=lam2[:, 1:2], in1=lam2[:, 0:1], op=ALU.subtract), reads=P_, writes=P_)
        p.op("dve", lambda e: e.tensor_scalar(out=nlam[:], in0=nlam[:], scalar1=-lam_init, scalar2=None, op0=ALU.add), reads=P_, writes=P_)
        p.op("dve", lambda e: e.tensor_scalar(out=qg[:], in0=qg[:], scalar1=0.125, scalar2=None, op0=ALU.mult), reads=P_, writes=P_)
        ps_n = p.psum("ps_n", [128, 512], F32)
        ps_s = [p.psum("ps_s%d" % i, [128, 128], F32) for i in range(2)]
        ps_o = [p.psum("ps_o%d" % i, [128, 256], F32) for i in range(2)]
        ps_t = p.psum("ps_t", [128, 128], F32)
        bpsn, bpst = p.buf(), p.buf()
        bpss = [p.buf(), p.buf()]
        bpso = [p.buf(), p.buf()]
        pt_sb = [p.sbuf("pt%d" % i, [128, 128], BF16) for i in range(2)]
        bpt = [p.buf(), p.buf()]
        o0 = p.sbuf("o0", [128, 128], F32)
        o1 = p.sbuf("o1", [128, 128], F32)
        rr = p.sbuf("rr", [128, 4], F32)
        bo = p.buf()
        p.op("dve", lambda e: e.memset(V1[:, :, 128:132], 1.0), writes=[bV1])
        for b in range(2):
            for cc in range(TB // 512 if TB >= 512 else 1):
                W_ = min(512, TB)
                g0 = b * TB + cc * W_
                for which, rowbase, gain, dstt, bd in ((0, 768, qg, qn, bqn), (1, 896, kg, kn, bkn)):
                    p.dma("sp", ld[:, 0:W_], PT.ap()[rowbase:rowbase + 128, g0:g0 + W_], reads=[b_PT], writes=[bld])
                    p.op("act", lambda e: e.activation(out=sqd[:, 0:W_], in_=ld[:, 0:W_], func=AF.Square), reads=[bld], writes=[bsqd])
                    p.op("pe", lambda e: e.matmul(ps_n[:, 0:W_], blk[:], sqd[:, 0:W_], start=True, stop=True),
                         reads=[bsqd, b_const], writes=[bpsn])
                    p.op("dve", lambda e: e.tensor_copy(out=rsd[:, 0:W_], in_=ps_n[:, 0:W_]), reads=[bpsn], writes=[brsd])
                    rstd_from(rsd[:, 0:W_], brsd, 1.0 / 64, 1e-6)
                    p.op("dve", lambda e: e.tensor_tensor(out=ld[:, 0:W_], in0=ld[:, 0:W_], in1=rsd[:, 0:W_], op=ALU.mult),
                         reads=[bld, brsd], writes=[bld])
                    p.op("dve", lambda e, gain=gain, dstt=dstt: e.tensor_scalar(
                        out=dstt[:, cc * W_:(cc + 1) * W_], in0=ld[:, 0:W_], scalar1=gain[:, 0:1], scalar2=None, op0=ALU.mult),
                        reads=[bld, bpar], writes=[bd])
                p.dma("sp", ld[:, 0:W_], PT.ap()[1024:1152, g0:g0 + W_], reads=[b_PT], writes=[bld])
                for t4 in range(W_ // 128):
                    transpose_to(ps_t[:, :], ld[:, t4 * 128:(t4 + 1) * 128], 128, [bld], bpst)
                    ti = cc * (W_ // 128) + t4
                    p.op("dve", lambda e, ti=ti: e.tensor_copy(out=V1[:, ti, 0:128], in_=ps_t[:, :]), reads=[bpst], writes=[bV1])
            cnt = 0
            for qt in range(NQT):
                qs = slice(qt * 128, (qt + 1) * 128)
                for c in range(2):
                    cs_ = slice(c * 64, (c + 1) * 64)

                    def st(kt, idx):
                        p.op("pe", lambda e: e.matmul(ps_s[idx][:, :], kn[cs_, kt * 128:(kt + 1) * 128], qn[cs_, qs],
                                                      start=True, stop=True), reads=[bkn, bqn], writes=[bpss[idx]])
                    st(0, cnt % 2)
                    for kt in range(qt + 1):
                        idx = cnt % 2
                        cnt += 1
                        if kt + 1 <= qt:
                            st(kt + 1, cnt % 2)
                        dl = qt - kt
                        p.op("act", lambda e, idx=idx, dl=dl: e.activation(out=pt_sb[idx][:], in_=ps_s[idx][:, :], func=AF.Exp,
                                                                         bias=abias[:, dl:dl + 1], scale=1.0),
                             reads=[bpss[idx], bpar], writes=[bpt[idx]])
                        if dl == 0:
                            p.op("dve", lambda e, idx=idx: e.tensor_tensor(out=pt_sb[idx][:], in0=pt_sb[idx][:], in1=triu[:], op=ALU.mult),
                                 reads=[bpt[idx], b_const], writes=[bpt[idx]])
                        p.op("pe", lambda e, idx=idx, kt=kt: e.matmul(ps_o[c][:, 0:129], pt_sb[idx][:], V1[:, kt, 0:129],
                                                                      start=(kt == 0), stop=(kt == qt)),
                             reads=[bpt[idx], bV1], writes=[bpso[c]])
                B_ = [bo]
                p.op("dve", lambda e: e.reciprocal(out=rr[:, 0:1], in_=ps_o[0][:, 128:129]), reads=[bpso[0]], writes=B_)
                p.op("dve", lambda e: e.reciprocal(out=rr[:, 1:2], in_=ps_o[1][:, 128:129]), reads=[bpso[1]], writes=B_)
                p.op("dve", lambda e: e.tensor_tensor(out=rr[:, 1:2], in0=rr[:, 1:2], in1=nlam[:], op=ALU.mult), reads=B_ + [bpar], writes=B_)
                p.op("dve", lambda e: e.tensor_scalar(out=o0[:], in0=ps_o[0][:, 0:128], scalar1=rr[:, 0:1], scalar2=None, op0=ALU.mult),
                     reads=B_ + [bpso[0]], writes=B_)
                p.op("dve", lambda e: e.scalar_tensor_tensor(out=o0[:], in0=ps_o[1][:, 0:128], scalar=rr[:, 1:2], in1=o0[:],
                                                             op0=ALU.mult, op1=ALU.add), reads=B_ + [bpso[1]], writes=B_)
                p.op("dve", lambda e: e.tensor_tensor(out=o1[:], in0=o0[:], in1=o0[:], op=ALU.mult), reads=B_, writes=B_)
                p.op("dve", lambda e: e.tensor_reduce(out=rr[:, 2:3], in_=o1[:], axis=AX.X, op=ALU.add), reads=B_, writes=B_)
                rstd_from(rr[:, 2:3], bo, 1.0 / 128, 1e-6)
                p.op("dve", lambda e: e.tensor_scalar(out=o0[:], in0=o0[:], scalar1=rr[:, 2:3], scalar2=(1.0 - lam_init),
                                                      op0=ALU.mult, op1=ALU.mult), reads=B_, writes=B_)
                p.op("dve", lambda e: e.tensor_tensor(out=o0[:], in0=o0[:], in1=sgt[:], op=ALU.mult), reads=B_ + [bpar], writes=B_)
                transpose_to(ps_t[:, :], o0[:], 128, B_, bpst)
                p.op("dve", lambda e, qs=qs: e.tensor_copy(out=oT[:, qs], in_=ps_t[:, :]), reads=[bpst], writes=[boT])
            p.dma_k("sp", oml[2].ap()[b * TB // 512:(b + 1) * TB // 512].rearrange("c p n -> p c n"), oT[:].rearrange("p (c n) -> p c n", n=512),
                    TB // 512, max(1, TB // 2048), reads=[boT], writes=[b_oml[2]])

        if stop_after == 'D':
            return src, src_b
        p.begin_phase()
        SBK = min(1024, TB)
        NCK = SBK // 64
        gcw = p.sbuf("gcw", [128, 12], F32)
        gsc = p.sbuf("gsc", [128, 2], F32)
        gngt = p.sbuf("gngt", [128, 128], F32)
        nega = p.sbuf("nega", [128, 1], F32)
        bgp = p.buf()
        p.dma("sp", gcw[:], smallp["gconv"].ap()[l], reads=[ext], writes=[bgp])
        p.dma("sp", gsc[:], smallp["gscal"].ap()[l], reads=[ext], writes=[bgp])
        p.dma("sp", gngt[:], smallp["gng"].ap()[l], reads=[ext], writes=[bgp])
        p.op("act", lambda e: e.activation(out=nega[:], in_=gsc[:, 0:1], func=AF.Exp), reads=[bgp], writes=[bgp])
        p.op("dve", lambda e: e.tensor_scalar(out=nega[:], in0=nega[:], scalar1=-1.0, scalar2=None, op0=ALU.mult), reads=[bgp], writes=[bgp])
        xin = p.sbuf("xin", [128, 3 + SBK], F32)
        qkv = [p.sbuf("qkv%d" % i, [128, SBK], F32) for i in range(3)]
        zt = p.sbuf("zt", [128, SBK], F32)
        oTg = p.sbuf("oTg", [128, SBK], F32)
        bxin, bz, boTg = p.buf(), p.buf(), p.buf()
        bqkv = [p.buf() for _ in range(3)]
        sqg = p.sbuf("sqg", [128, SBK], F32)
        rsg = p.sbuf("rsg", [128, SBK], F32)
        bsqg, brsg = p.buf(), p.buf()
        gtm = p.sbuf("gtm", [64, NCK], F32)
        btm = p.sbuf("btm", [64, NCK], F32)
        nbt = p.sbuf("nbt", [64, NCK], F32)
        Gtm = p.sbuf("Gtm", [64, NCK], F32)
        eG = p.sbuf("eG", [64, NCK], F32)
        tl = p.sbuf("tl", [64, NCK], F32)
        bge = p.sbuf("bge", [64, NCK], F32)
        dch = p.sbuf("dch", [128, NCK], F32)
        bsc = p.buf()
        S = p.sbuf("S", [128, 128], F32)
        bS = p.buf()
        NPS = 6
        ps_g2 = p.psum("ps_g2", [128, 512], F32)
        bpsg2 = p.buf()
        pool = [p.psum("gps%d" % i, [128, 128], F32) for i in range(NPS)]
        bpool = [p.buf() for _ in range(NPS)]
        pc = [0]

        def ps():
            i = pc[0] % NPS
            pc[0] += 1
            return pool[i], bpool[i]
        names = ["k_tm", "kbg", "ktail", "vb", "z_tm", "e1", "P", "PT_", "P2", "PT2", "IP", "X", "X2", "u", "wT", "vnew", "qkT", "o", "o2", "gm1", "gm2", "e2", "red"]
        T = {}
        Bf = {}
        for nm in names:
            T[nm] = p.sbuf("g_" + nm, [128, 128], F32)
            Bf[nm] = p.buf()
        I64 = ident_sb[0:64, 0:64]
        for b in range(2):
            p.op("dve", lambda e: e.memset(S[:], 0.0), writes=[bS])
            for sb in range(TB // SBK):
                g0 = b * TB + sb * SBK
                for w in range(3):
                    if sb == 0:
                        p.op("dve", lambda e: e.memset(xin[:, 0:3], 0.0), writes=[bxin])
                        p.dma("sp", xin[:, 3:3 + SBK], PT.ap()[w * 128:(w + 1) * 128, g0:g0 + SBK], reads=[b_PT], writes=[bxin])
                    else:
                        p.dma("sp", xin[:], PT.ap()[w * 128:(w + 1) * 128, g0 - 3:g0 + SBK], reads=[b_PT], writes=[bxin])
                    t_ = qkv[w]
                    p.op("dve", lambda e, w=w, t_=t_: e.tensor_scalar(out=t_[:], in0=xin[:, 3:3 + SBK], scalar1=gcw[:, w * 4 + 3:w * 4 + 4],
                                                                     scalar2=None, op0=ALU.mult), reads=[bxin, bgp], writes=[bqkv[w]])
                    for j in range(3):
                        p.op("dve", lambda e, w=w, j=j, t_=t_: e.scalar_tensor_tensor(
                            out=t_[:], in0=xin[:, j:j + SBK], scalar=gcw[:, w * 4 + j:w * 4 + j + 1], in1=t_[:],
                            op0=ALU.mult, op1=ALU.add), reads=[bxin, bgp, bqkv[w]], writes=[bqkv[w]])
                    p.op("act", lambda e, t_=t_: e.activation(out=t_[:], in_=t_[:], func=AF.Silu), reads=[bqkv[w]], writes=[bqkv[w]])
                for w, scl in ((0, 128.0 ** -0.5), (1, 1.0)):
                    t_ = qkv[w]
                    p.op("act", lambda e, t_=t_: e.activation(out=sqg[:], in_=t_[:], func=AF.Square), reads=[bqkv[w]], writes=[bsqg])
                    for hh in range((SBK + 511) // 512):
                        ws_ = slice(hh * 512, min(SBK, (hh + 1) * 512))
                        wl = ws_.stop - ws_.start
                        p.op("pe", lambda e, ws_=ws_, wl=wl: e.matmul(ps_g2[:, 0:wl], ones_f[:], sqg[:, ws_], start=True, stop=True),
                             reads=[bsqg, b_const], writes=[bpsg2])
                        p.op("dve", lambda e, ws_=ws_, wl=wl: e.tensor_copy(out=rsg[:, ws_], in_=ps_g2[:, 0:wl]), reads=[bpsg2], writes=[brsg])
                    rstd_from(rsg[:], brsg, 1.0, 1e-6)
                    p.op("dve", lambda e, t_=t_, scl=scl: e.scalar_tensor_tensor(out=t_[:], in0=t_[:], scalar=scl, in1=rsg[:],
                                                                               op0=ALU.mult, op1=ALU.mult), reads=[bqkv[w], brsg], writes=[bqkv[w]])
                p.dma("sp", zt[:], PT.ap()[384:512, g0:g0 + SBK], reads=[b_PT], writes=[bz])
                p.op("act", lambda e: e.activation(out=zt[:], in_=zt[:], func=AF.Silu), reads=[bz], writes=[bz])
                SC = [bsc]
                p.dma("sp", btm[:], PT.ap()[1152, g0:g0 + SBK].rearrange("(c i) -> i c", i=64), reads=[b_PT], writes=SC,
                      allow_slow_non_contiguous=True)
                p.dma("sp", gtm[:], PT.ap()[1153, g0:g0 + SBK].rearrange("(c i) -> i c", i=64), reads=[b_PT], writes=SC,
                      allow_slow_non_contiguous=True)
                p.op("act", lambda e: e.activation(out=btm[:], in_=btm[:], func=AF.Sigmoid), reads=SC, writes=SC)
                p.op("dve", lambda e: e.tensor_scalar(out=nbt[:], in0=btm[:], scalar1=-1.0, scalar2=None, op0=ALU.mult), reads=SC, writes=SC)
                p.op("act", lambda e: e.activation(out=gtm[:], in_=gtm[:], func=AF.Exp, bias=gsc[0:64, 1:2], scale=1.0), reads=SC + [bgp], writes=SC)
                p.op("act", lambda e: e.activation(out=gtm[:], in_=gtm[:], func=AF.Ln, bias=1.0, scale=1.0), reads=SC, writes=SC)
                p.op("dve", lambda e: e.tensor_scalar(out=gtm[:], in0=gtm[:], scalar1=nega[0:64, 0:1], scalar2=None, op0=ALU.mult), reads=SC + [bgp], writes=SC)
                pst, bpst_ = ps()
                p.op("pe", lambda e: e.matmul(pst[0:64, 0:NCK], triu[0:64, 0:64], gtm[:], start=True, stop=True), reads=SC + [b_const], writes=[bpst_])
                p.op("dve", lambda e: e.tensor_copy(out=Gtm[:], in_=pst[0:64, 0:NCK]), reads=[bpst_], writes=SC)
                p.op("act", lambda e: e.activation(out=eG[:], in_=Gtm[:], func=AF.Exp), reads=SC, writes=SC)
                pst, bpst_ = ps()
                p.op("pe", lambda e: e.matmul(pst[:, 0:NCK], sel63[:, :], Gtm[:], start=True, stop=True), reads=SC + [b_const], writes=[bpst_])
                p.op("act", lambda e: e.activation(out=dch[:], in_=pst[:, 0:NCK], func=AF.Exp), reads=[bpst_], writes=SC)
                p.op("dve", lambda e: e.tensor_tensor(out=tl[:], in0=pst[0:64, 0:NCK], in1=Gtm[:], op=ALU.subtract), reads=[bpst_] + SC, writes=SC)
                p.op("act", lambda e: e.activation(out=tl[:], in_=tl[:], func=AF.Exp), reads=SC, writes=SC)
                p.op("dve", lambda e: e.tensor_tensor(out=bge[:], in0=btm[:], in1=eG[:], op=ALU.mult), reads=SC, writes=SC)
                qT_, kT_, vT_ = qkv
                for ci in range(NCK):
                    cs = slice(ci * 64, (ci + 1) * 64)
                    col = slice(ci, ci + 1)
                    RK, RQ, RV = [bqkv[1]], [bqkv[0]], [bqkv[2]]
                    pa, ba = ps()
                    transpose_to(pa[0:64, :], kT_[:, cs], 128, RK, ba)
                    p.op("dve", lambda e: e.tensor_scalar(out=T["kbg"][0:64, :], in0=pa[0:64, :], scalar1=bge[:, col], scalar2=None, op0=ALU.mult),
                         reads=[ba] + SC, writes=[Bf["kbg"]])
                    p.op("dve", lambda e: e.tensor_scalar(out=T["ktail"][0:64, :], in0=pa[0:64, :], scalar1=tl[:, col], scalar2=None, op0=ALU.mult),
                         reads=[ba] + SC, writes=[Bf["ktail"]])
                    pa, ba = ps()
                    transpose_to(pa[0:64, :], vT_[:, cs], 128, RV, ba)
                    p.op("dve", lambda e: e.tensor_scalar(out=T["vb"][0:64, :], in0=pa[0:64, :], scalar1=btm[:, col], scalar2=None, op0=ALU.mult),
                         reads=[ba] + SC, writes=[Bf["vb"]])
                    pa, ba = ps()
                    transpose_to(pa[0:64, :], zt[:, cs], 128, [bz], ba)
                    p.op("dve", lambda e: e.tensor_copy(out=T["z_tm"][0:64, :], in_=pa[0:64, :]), reads=[ba], writes=[Bf["z_tm"]])
                    p.op("dve", lambda e: e.tensor_scalar(out=T["gm1"][0:64, 0:64], in0=triu[0:64, 0:64], scalar1=gtm[:, col], scalar2=None, op0=ALU.mult),
                         reads=SC + [b_const], writes=[Bf["gm1"]])
                    p.op("dve", lambda e: e.tensor_scalar(out=T["gm2"][0:64, 0:64], in0=trils[0:64, 0:64], scalar1=gtm[:, col], scalar2=None, op0=ALU.mult),
                         reads=SC + [b_const], writes=[Bf["gm2"]])
                    pD, bD = ps()
                    p.op("pe", lambda e: e.matmul(pD[0:64, 0:64], T["gm1"][0:64, 0:64], trils[0:64, 0:64], start=True, stop=True),
                         reads=[Bf["gm1"], b_const], writes=[bD])
                    pG, bG = ps()
                    p.op("pe", lambda e: e.matmul(pG[0:64, 0:64], kT_[:, cs], kT_[:, cs], start=True, stop=True), reads=RK, writes=[bG])
                    p.op("act", lambda e: e.activation(out=T["e1"][0:64, 0:64], in_=pD[0:64, 0:64], func=AF.Exp), reads=[bD], writes=[Bf["e1"]])
                    p.op("dve", lambda e: e.tensor_tensor(out=T["e1"][0:64, 0:64], in0=T["e1"][0:64, 0:64], in1=trils[0:64, 0:64], op=ALU.mult),
                         reads=[Bf["e1"], b_const], writes=[Bf["e1"]])
                    p.op("dve", lambda e: e.scalar_tensor_tensor(out=T["P"][0:64, 0:64], in0=pG[0:64, 0:64], scalar=nbt[:, col], in1=T["e1"][0:64, 0:64],
                                                                 op0=ALU.mult, op1=ALU.mult), reads=[bG, Bf["e1"]] + SC, writes=[Bf["P"]])
                    pD2, bD2 = ps()
                    p.op("pe", lambda e: e.matmul(pD2[0:64, 0:64], T["gm2"][0:64, 0:64], triu[0:64, 0:64], start=True, stop=True),
                         reads=[Bf["gm2"], b_const], writes=[bD2])
                    pKQ, bKQ = ps()
                    p.op("pe", lambda e: e.matmul(pKQ[0:64, 0:64], kT_[:, cs], qT_[:, cs], start=True, stop=True), reads=RK + RQ, writes=[bKQ])
                    p.op("act", lambda e: e.activation(out=T["e2"][0:64, 0:64], in_=pD2[0:64, 0:64], func=AF.Exp), reads=[bD2], writes=[Bf["e2"]])
                    p.op("dve", lambda e: e.tensor_tensor(out=T["e2"][0:64, 0:64], in0=T["e2"][0:64, 0:64], in1=triu[0:64, 0:64], op=ALU.mult),
                         reads=[Bf["e2"], b_const], writes=[Bf["e2"]])
                    p.op("dve", lambda e: e.tensor_tensor(out=T["qkT"][0:64, 0:64], in0=pKQ[0:64, 0:64], in1=T["e2"][0:64, 0:64], op=ALU.mult),
                         reads=[bKQ, Bf["e2"]], writes=[Bf["qkT"]])
                    pa, ba = ps()
                    transpose_to(pa[0:64, 0:64], T["P"][0:64, 0:64], 64, [Bf["P"]], ba)
                    p.op("dve", lambda e: e.tensor_copy(out=T["PT_"][0:64, 0:64], in_=pa[0:64, 0:64]), reads=[ba], writes=[Bf["PT_"]])
                    p.op("dve", lambda e: e.tensor_tensor(out=T["X"][0:64, 0:64], in0=pa[0:64, 0:64], in1=I64, op=ALU.add),
                         reads=[ba, b_const], writes=[Bf["X"]])
                    Pc, PTc, Xc = "P", "PT_", "X"
                    Pn, PTn, Xn = "P2", "PT2", "X2"
                    for lev in range(5):
                        pa, ba = ps()
                        p.op("pe", lambda e: e.matmul(pa[0:64, 0:64], T[PTc][0:64, 0:64], T[Pc][0:64, 0:64], start=True, stop=True),
                             reads=[Bf[PTc], Bf[Pc]], writes=[ba])
                        pb, bb = ps()
                        p.op("pe", lambda e: e.matmul(pb[0:64, 0:64], T[Pc][0:64, 0:64], T[PTc][0:64, 0:64], start=True, stop=True),
                             reads=[Bf[PTc], Bf[Pc]], writes=[bb])
                        p.op("dve", lambda e: e.tensor_copy(out=T[Pn][0:64, 0:64], in_=pa[0:64, 0:64]), reads=[ba], writes=[Bf[Pn]])
                        p.op("dve", lambda e: e.tensor_tensor(out=T["IP"][0:64, 0:64], in0=pa[0:64, 0:64], in1=I64, op=ALU.add),
                             reads=[ba, b_const], writes=[Bf["IP"]])
                        p.op("act", lambda e: e.activation(out=T[PTn][0:64, 0:64], in_=pb[0:64, 0:64], func=AF.Copy), reads=[bb], writes=[Bf[PTn]])
                        pc_, bc_ = ps()
                        p.op("pe", lambda e: e.matmul(pc_[0:64, 0:64], T["IP"][0:64, 0:64], T[Xc][0:64, 0:64], start=True, stop=True),
                             reads=[Bf["IP"], Bf[Xc]], writes=[bc_])
                        p.op("dve", lambda e: e.tensor_copy(out=T[Xn][0:64, 0:64], in_=pc_[0:64, 0:64]), reads=[bc_], writes=[Bf[Xn]])
                        Pc, Pn = Pn, Pc
                        PTc, PTn = PTn, PTc
                        Xc, Xn = Xn, Xc
                    pu, bu = ps()
                    p.op("pe", lambda e: e.matmul(pu[0:64, :], T[Xc][0:64, 0:64], T["vb"][0:64, :], start=True, stop=True),
                         reads=[Bf[Xc], Bf["vb"]], writes=[bu])
                    p.op("act", lambda e: e.activation(out=T["u"][0:64, :], in_=pu[0:64, :], func=AF.Copy), reads=[bu], writes=[Bf["u"]])
                    pw, bw = ps()
                    p.op("pe", lambda e: e.matmul(pw[:, 0:64], T["kbg"][0:64, :], T[Xc][0:64, 0:64], start=True, stop=True),
                         reads=[Bf[Xc], Bf["kbg"]], writes=[bw])
                    p.op("dve", lambda e: e.tensor_copy(out=T["wT"][:, 0:64], in_=pw[:, 0:64]), reads=[bw], writes=[Bf["wT"]])
                    pws, bws = ps()
                    p.op("pe", lambda e: e.matmul(pws[0:64, :], T["wT"][:, 0:64], S[:], start=True, stop=True), reads=[Bf["wT"], bS], writes=[bws])
                    p.op("dve", lambda e: e.tensor_tensor(out=T["vnew"][0:64, :], in0=T["u"][0:64, :], in1=pws[0:64, :], op=ALU.subtract),
                         reads=[Bf["u"], bws], writes=[Bf["vnew"]])
                    po1, bo1 = ps()
                    p.op("pe", lambda e: e.matmul(po1[0:64, :], qT_[:, cs], S[:], start=True, stop=True), reads=RQ + [bS], writes=[bo1])
                    po2, bo2 = ps()
                    p.op("pe", lambda e: e.matmul(po2[0:64, :], T["qkT"][0:64, 0:64], T["vnew"][0:64, :], start=True, stop=True),
                         reads=[Bf["qkT"], Bf["vnew"]], writes=[bo2])
                    p.op("dve", lambda e: e.tensor_scalar(out=T["o"][0:64, :], in0=po1[0:64, :], scalar1=eG[:, col], scalar2=None, op0=ALU.mult),
                         reads=[bo1] + SC, writes=[Bf["o"]])
                    p.op("dve", lambda e: e.tensor_tensor(out=T["o"][0:64, :], in0=T["o"][0:64, :], in1=po2[0:64, :], op=ALU.add),
                         reads=[Bf["o"], bo2], writes=[Bf["o"]])
                    pS, bpS = ps()
                    p.op("pe", lambda e: e.matmul(pS[:, :], T["ktail"][0:64, :], T["vnew"][0:64, :], start=True, stop=True),
                         reads=[Bf["ktail"], Bf["vnew"]], writes=[bpS])
                    p.op("dve", lambda e: e.scalar_tensor_tensor(out=S[:], in0=S[:], scalar=dch[:, col], in1=pS[:, :], op0=ALU.mult, op1=ALU.add),
                         reads=[bS, bpS] + SC, writes=[bS])
                    O_ = [Bf["o"]]
                    p.op("dve", lambda e: e.tensor_tensor(out=T["o2"][0:64, :], in0=T["o"][0:64, :], in1=T["o"][0:64, :], op=ALU.mult), reads=O_, writes=[Bf["o2"]])
                    p.op("dve", lambda e: e.tensor_reduce(out=T["red"][0:64, 0:1], in_=T["o2"][0:64, :], axis=AX.X, op=ALU.add), reads=[Bf["o2"]], writes=[Bf["red"]])
                    rstd_from(T["red"][0:64, 0:1], Bf["red"], 1.0 / 128, 1e-6)
                    p.op("dve", lambda e: e.scalar_tensor_tensor(out=T["o"][0:64, :], in0=T["o"][0:64, :], scalar=T["red"][0:64, 0:1], in1=gngt[0:64, :],
                                                                 op0=ALU.mult, op1=ALU.mult), reads=O_ + [Bf["red"], bgp], writes=O_)
                    p.op("dve", lambda e: e.tensor_tensor(out=T["o"][0:64, :], in0=T["o"][0:64, :], in1=T["z_tm"][0:64, :], op=ALU.mult),
                         reads=O_ + [Bf["z_tm"]], writes=O_)
                    pa, ba = ps()
                    transpose_to(pa[:, 0:64], T["o"][0:64, :], 64, O_, ba)
                    p.op("dve", lambda e: e.tensor_copy(out=oTg[:, cs], in_=pa[:, 0:64]), reads=[ba], writes=[boTg])
                p.dma("sp", oml[0].ap()[g0 // 512:(g0 + SBK) // 512].rearrange("c p n -> p c n"), oTg[:].rearrange("p (c n) -> p c n", n=512),
                      reads=[boTg], writes=[b_oml[0]])

        if stop_after == 'E':
            return src, src_b
        p.begin_phase()
        for m in range(3):
            for c in range(NTOT // 512):
                p.allgather(oml[m].ap()[c], Gm[m].ap()[c].rearrange("r j f -> (r j) f"),
                            reads=[b_oml[m]], writes=[b_Gm[m]])
        A1, B1, G1, bm1 = load_mod3(l, 0, norm1_g)
        lng = p.sbuf("lng", [128, 8], F32)
        lnb = p.sbuf("lnb", [128, 8], F32)
        blp = p.buf()
        p.dma("sp", lng[:], smallp["clng"].ap()[l], reads=[ext], writes=[blp])
        p.dma("sp", lnb[:], smallp["clnb"].ap()[l], reads=[ext], writes=[blp])
        hbF = p.sbuf("hbF", [128, KT, TC], BF16)
        of32 = [p.sbuf("of%d" % m, [128, 8, TC], F32) for m in range(3)]
        obf = [p.sbuf("obf%d" % m, [128, 8, TC], BF16) for m in range(3)]
        mixb = p.sbuf("mixb", [128, KT, TC], BF16)
        mtmp = p.sbuf("mtmp", [128, 4, TC], F32)
        sgF = p.sbuf("sgF", [128, TC], F32)
        st1 = p.sbuf("st1", [128, TC], F32)
        st2 = p.sbuf("st2", [128, TC], F32)
        xt = [p.sbuf("xt%d" % i, [128, TC], F32) for i in range(2)]
        wob = [p.sbuf("wob%d" % i, [128, 8, 512], BF16) for i in range(2)]
        bhbF, bmix, bmt, bsgF, bst = p.buf(), p.buf(), p.buf(), p.buf(), p.buf()
        bof = [p.buf() for _ in range(3)]
        bobf = [p.buf() for _ in range(3)]
        bxt = [p.buf(), p.buf()]
        bwob = [p.buf(), p.buf()]
        psg2_ = [p.psum("psgF%d" % i, [128, TC], F32) for i in range(2)]
        psy2_ = [p.psum("psyF%d" % i, [128, TC], F32) for i in range(2)]
        psg = [psg2_[i % 2] for i in range(4)]
        psy = [psy2_[i % 2] for i in range(4)]
        pst2 = p.psum("pst2", [128, 512], F32)
        bpsg2_ = [p.buf() for _ in range(2)]
        bpsy2_ = [p.buf() for _ in range(2)]
        bpsg = [bpsg2_[i % 2] for i in range(4)]
        bpsy = [bpsy2_[i % 2] for i in range(4)]
        bpst2 = p.buf()
        nwo = 0
        nxt = 0
        Gm_rows = [Gm[m].ap().rearrange("c r j (tb f) -> (c r j tb) f", f=TC) for m in range(3)]
        nrows = (NTOT // 512) * NCORES * 128 * (512 // TC)
        for ch in range(NT // TC):
            t0 = ch * TC
            p.dma_k("sp", hbF[:], hloc.ap()[t0 // TG].rearrange("(k p) n -> p k n", p=128)[:, :, t0 % TG:t0 % TG + TC], KT, 4, reads=[b_hloc], writes=[bhbF])
            for m in range(3):
                for s_ in range(NCORES):
                    g0s = s_ * NT + t0
                    c_, tb_ = g0s // 512, (g0s % 512) // TC
                    sw = nload % 2
                    nload += 1
                    cand = wbuf[sw][:].bitcast(F32)[:, 0:8 * TC].rearrange("p (h n) -> p h n", h=8)
                    p.dma("sp", cand, Gm[m].ap()[c_, :, :, tb_ * TC:(tb_ + 1) * TC].rearrange("h p n -> p h n"),
                          reads=[b_Gm[m]], writes=[wbb[sw]])
                    if s_ == 0:
                        p.op("dve", lambda e, cand=cand, s_=s_: e.tensor_scalar(out=of32[m][:], in0=cand, scalar1=rsel_sb[:, s_:s_ + 1],
                                                                             scalar2=None, op0=ALU.mult), reads=[wbb[sw], b_const], writes=[bof[m]])
                    else:
                        p.op("dve", lambda e, cand=cand, s_=s_: e.scalar_tensor_tensor(out=of32[m][:], in0=cand, scalar=rsel_sb[:, s_:s_ + 1],
                                                                                    in1=of32[m][:], op0=ALU.mult, op1=ALU.add),
                             reads=[wbb[sw], b_const, bof[m]], writes=[bof[m]])
            for k in range(8):
                p.op("pe", lambda e, k=k: e.matmul(pst2[:, 0:TC], ones_f[:], of32[1][:, k, :], start=(k == 0), stop=(k == 7)),
                     reads=[bof[1], b_const], writes=[bpst2])
            p.op("dve", lambda e: e.tensor_scalar(out=st1[:], in0=pst2[:, 0:TC], scalar1=1.0 / 1024, scalar2=None, op0=ALU.mult),
                 reads=[bpst2], writes=[bst])
            for k in range(8):
                p.op("dve", lambda e, k=k: e.tensor_tensor(out=of32[1][:, k, :], in0=of32[1][:, k, :], in1=st1[:], op=ALU.subtract),
                     reads=[bof[1], bst], writes=[bof[1]])
                p.op("act", lambda e, k=k: e.activation(out=sgF[:], in_=of32[1][:, k, :], func=AF.Square), reads=[bof[1]], writes=[bsgF])
                p.op("pe", lambda e, k=k: e.matmul(pst2[:, 0:TC], ones_f[:], sgF[:], start=(k == 0), stop=(k == 7)),
                     reads=[bsgF, b_const], writes=[bpst2])
            p.op("dve", lambda e: e.tensor_copy(out=st2[:], in_=pst2[:, 0:TC]), reads=[bpst2], writes=[bst])
            rstd_from(st2[:], bst, 1.0 / 1024, 1e-6)
            for k in range(8):
                p.op("dve", lambda e, k=k: e.tensor_tensor(out=of32[1][:, k, :], in0=of32[1][:, k, :], in1=st2[:], op=ALU.mult),
                     reads=[bof[1], bst], writes=[bof[1]])
                p.op("dve", lambda e, k=k: e.tensor_scalar(out=of32[1][:, k, :], in0=of32[1][:, k, :], scalar1=lng[:, k:k + 1],
                                                           scalar2=lnb[:, k:k + 1], op0=ALU.mult, op1=ALU.add), reads=[bof[1], blp], writes=[bof[1]])
            p.op("act", lambda e: e.activation(out=obf[1][:], in_=of32[1][:], func=AF.Silu), reads=[bof[1]], writes=[bobf[1]])
            p.op("act", lambda e: e.activation(out=obf[0][:], in_=of32[0][:], func=AF.Copy), reads=[bof[0]], writes=[bobf[0]])
            p.op("dve", lambda e: e.tensor_copy(out=obf[2][:], in_=of32[2][:]), reads=[bof[2]], writes=[bobf[2]])
            for dg in range(8):
                for m in range(3):
                    s = nload % 2
                    nload += 1
                    wv = wbuf[s][:].rearrange("p (k n) -> p k n", k=KT)
                    p.dma_k("sp", wv, gg.ap().rearrange("(k p) n -> p k n", p=128)[:, :, m * D + dg * 512:m * D + (dg + 1) * 512], KT, 4,
                            reads=[ggb], writes=[wbb[s]])
                    s2 = nwo % 2
                    nwo += 1
                    p.dma("sp", wob[s2][:], ga[m][0].ap().rearrange("(k p) n -> p k n", p=128)[:, :, dg * 512:(dg + 1) * 512],
                          reads=[ga[m][1]], writes=[bwob[s2]])
                    for ft in range(4):
                        for k in range(KT):
                            p.op("pe", lambda e, k=k, ft=ft, wv=wv: e.matmul(psg[ft][:], wv[:, k, ft * 128:(ft + 1) * 128], hbF[:, k, :],
                                                                          start=(k == 0), stop=(k == KT - 1)), reads=[wbb[s], bhbF], writes=[bpsg[ft]])
                        for k in range(8):
                            p.op("pe", lambda e, k=k, ft=ft, s2=s2, m=m: e.matmul(psy[ft][:], wob[s2][:, k, ft * 128:(ft + 1) * 128], obf[m][:, k, :],
                                                                                start=(k == 0), stop=(k == 7)), reads=[bwob[s2], bobf[m]], writes=[bpsy[ft]])
                        p.op("act", lambda e, ft=ft: e.activation(out=sgF[:], in_=psg[ft][:], func=AF.Sigmoid), reads=[bpsg[ft]], writes=[bsgF])
                        if m == 0:
                            p.op("dve", lambda e, ft=ft: e.tensor_tensor(out=mtmp[:, ft, :], in0=sgF[:], in1=psy[ft][:], op=ALU.mult),
                                 reads=[bsgF, bpsy[ft]], writes=[bmt])
                        else:
                            p.op("dve", lambda e, ft=ft: e.tensor_tensor(out=sgF[:], in0=sgF[:], in1=psy[ft][:], op=ALU.mult),
                                 reads=[bsgF, bpsy[ft]], writes=[bsgF])
                            if m == 1:
                                p.op("dve", lambda e, ft=ft: e.tensor_tensor(out=mtmp[:, ft, :], in0=mtmp[:, ft, :], in1=sgF[:], op=ALU.add),
                                     reads=[bsgF, bmt], writes=[bmt])
                            else:
                                p.op("dve", lambda e, ft=ft, dg=dg: e.tensor_tensor(out=mixb[:, dg * 4 + ft, :], in0=mtmp[:, ft, :], in1=sgF[:], op=ALU.add),
                                     reads=[bsgF, bmt], writes=[bmix])
            for dg in range(8):
                s = nload % 2
                nload += 1
                wv = wbuf[s][:].rearrange("p (k n) -> p k n", k=KT)
                p.dma_k("sp", wv, go.ap().rearrange("(k p) n -> p k n", p=128)[:, :, dg * 512:(dg + 1) * 512], KT, 4, reads=[gob], writes=[wbb[s]])
                for ft in range(4):
                    di = dg * 4 + ft
                    xi = nxt % 2
                    nxt += 1
                    p.dma("sp", xt[xi][:], src.ap()[di * 128:(di + 1) * 128, t0:t0 + TC], reads=[src_b], writes=[bxt[xi]])
                    for k in range(KT):
                        p.op("pe", lambda e, k=k, ft=ft, wv=wv: e.matmul(psg[ft][:], wv[:, k, ft * 128:(ft + 1) * 128], mixb[:, k, :],
                                                                      start=(k == 0), stop=(k == KT - 1)), reads=[wbb[s], bmix], writes=[bpsg[ft]])
                    p.op("dve", lambda e, ft=ft, xi=xi, di=di: e.scalar_tensor_tensor(out=xt[xi][:], in0=psg[ft][:], scalar=G1[:, di:di + 1], in1=xt[xi][:],
                                                                                   op0=ALU.mult, op1=ALU.add), reads=[bpsg[ft], bm1, bxt[xi]], writes=[bxt[xi]])
                    p.dma("sp", x1.ap()[di * 128:(di + 1) * 128, t0:t0 + TC], xt[xi][:], reads=[bxt[xi]], writes=[b_x1])
        return x1, b_x1

    def moe_layer(l, src, src_b, dst, dst_b):
        nonlocal nload
        p.begin_phase()
        x_sb = p.sbuf("x_sb", [128, KT, TC], F32)
        hf_sb = p.sbuf("hf_sb", [128, KT, TC], F32)
        h_bf = p.sbuf("h_bf", [128, KT, TC], BF16)
        sq_sb = p.sbuf("sq_sb", [128, TC], F32)
        rstd_sb = p.sbuf("rstd_sb", [128, TC], F32)
        hid_sb = [p.sbuf("hid%d" % i, [128, 8, TC], BF16) for i in range(2)]
        sg_sb = p.sbuf("sg_sb", [128, TC], F32)
        cb_sb = [p.sbuf("cb%d" % i, [128, TC], F32) for i in range(2)]
        modc_sb = p.sbuf("modc_sb", [128, 3, KT, 2], F32)
        A_sb = p.sbuf("A_sb", [128, KT], F32)
        B_sb = p.sbuf("B_sb", [128, KT], F32)
        G_sb = p.sbuf("G_sb", [128, KT], F32)
        g2_sb = p.sbuf("g2_sb", [128, KT], F32)
        tmpk = p.sbuf("tmpk", [128, KT], F32)
        r_sc = p.sbuf("r_sc", [128, NE], F32)
        r_sel = p.sbuf("r_sel", [128, NE], F32)
        r_t1 = p.sbuf("r_t1", [128, NE], F32)
        r_t2 = p.sbuf("r_t2", [128, NE], F32)
        r_g1 = p.sbuf("r_g1", [128, 4], F32)
        r_g2 = p.sbuf("r_g2", [128, 4], F32)
        r_m = p.sbuf("r_m", [128, 1], F32)
        r_cmb = p.sbuf("r_cmb", [128, NE], F32)
        r_cT = p.sbuf("r_cT", [NE, TC], F32)
        combT = p.dramu("combT", [NE, TC], F32)
        ps_ss = p.psum("ps_ss", [128, 512], F32)
        ps_g = [p.psum("ps_g%d" % i, [128, TC], F32) for i in range(2)]
        ps_u = [p.psum("ps_u%d" % i, [128, TC], F32) for i in range(2)]
        ps_d = [p.psum("ps_d%d" % i, [128, TC], F32) for i in range(2)]
        b_x, b_hf, b_hbf, b_sq, b_rstd = p.buf("x"), p.buf("hf"), p.buf("hbf"), p.buf("sq"), p.buf("rstd")
        b_hid = [p.buf(), p.buf()]
        b_sg = p.buf("sg")
        b_cb = [p.buf(), p.buf()]
        b_mod = p.buf("mod")
        b_r = p.buf("router")
        b_rcT = p.buf("rcT")
        b_combT = p.buf("combT")
        b_pss = p.buf("ps_ss")
        b_psg = [p.buf(), p.buf()]
        b_psu = [p.buf(), p.buf()]
        b_psd = [p.buf(), p.buf()]

        g1, bg1, g2, bg2, g3, bg3 = gw[l]
        mf = mod_full[l].ap().rearrange("(r p) (c b) -> p r c b", p=128, b=2)
        for ci in range(3):
            t = (3 + ci) * 32
            tend = t + 32
            while t < tend:
                r_, c_ = t // 24, t % 24
                n = min(24 - c_, tend - t)
                k0 = t - (3 + ci) * 32
                p.dma("sp", modc_sb[:, ci, k0:k0 + n, :], mf[:, r_, c_:c_ + n, :], reads=[mod_full_b[l]], writes=[b_mod])
                t += n
        p.dma("sp", g2_sb[:], norm2_g.ap()[l], reads=[ext], writes=[b_mod])

        def pick(out_t, ci):
            p.op("dve", lambda e: e.tensor_scalar(out=tmpk[:], in0=modc_sb[:, ci, :, 1], scalar1=bsel_sb[:, 1:2],
                                                  scalar2=None, op0=ALU.mult), reads=[b_mod, b_const], writes=[b_mod])
            p.op("dve", lambda e: e.scalar_tensor_tensor(out=out_t[:], in0=modc_sb[:, ci, :, 0], scalar=bsel_sb[:, 0:1],
                                                         in1=tmpk[:], op0=ALU.mult, op1=ALU.add),
                 reads=[b_mod, b_const], writes=[b_mod])
        pick(B_sb, 0)
        pick(A_sb, 1)
        pick(G_sb, 2)
        p.op("dve", lambda e: e.scalar_tensor_tensor(out=A_sb[:], in0=A_sb[:], scalar=1.0, in1=g2_sb[:],
                                                     op0=ALU.add, op1=ALU.mult), reads=[b_mod], writes=[b_mod])
        for ch in range(NCH):
            t0 = ch * TC
            p.dma_k("sp", x_sb[:], src.ap().rearrange("(k p) n -> p k n", p=128)[:, :, t0:t0 + TC], KT, 4,
                    reads=[src_b], writes=[b_x])
            for k in range(KT):
                p.op("act", lambda e, k=k: e.activation(out=sq_sb[:], in_=x_sb[:, k, :], func=AF.Square),
                     reads=[b_x], writes=[b_sq])
                p.op("pe", lambda e, k=k: e.matmul(ps_ss[:, 0:TC], ones_f[:], sq_sb[:], start=(k == 0), stop=(k == KT - 1)),
                     reads=[b_sq, b_const], writes=[b_pss])
            p.op("dve", lambda e: e.tensor_scalar(out=rstd_sb[:], in0=ps_ss[:, 0:TC], scalar1=1.0 / D, scalar2=1e-6,
                                                  op0=ALU.mult, op1=ALU.add), reads=[b_pss], writes=[b_rstd])
            p.op("act", lambda e: e.activation(out=rstd_sb[:], in_=rstd_sb[:], func=AF.Ln), reads=[b_rstd], writes=[b_rstd])
            p.op("act", lambda e: e.activation(out=rstd_sb[:], in_=rstd_sb[:], func=AF.Exp, scale=-0.5),
                 reads=[b_rstd], writes=[b_rstd])
            for k in range(KT):
                p.op("dve", lambda e, k=k: e.tensor_tensor(out=hf_sb[:, k, :], in0=x_sb[:, k, :], in1=rstd_sb[:], op=ALU.mult),
                     reads=[b_x, b_rstd], writes=[b_hf])
                p.op("dve", lambda e, k=k: e.tensor_scalar(out=hf_sb[:, k, :], in0=hf_sb[:, k, :], scalar1=A_sb[:, k:k + 1],
                                                           scalar2=B_sb[:, k:k + 1], op0=ALU.mult, op1=ALU.add),
                     reads=[b_hf, b_mod], writes=[b_hf])
            p.op("act", lambda e: e.activation(out=h_bf[:], in_=hf_sb[:], func=AF.Copy), reads=[b_hf], writes=[b_hbf])
            for tt in range(TC // 128):
                ts = slice(tt * 128, (tt + 1) * 128)
                for k in range(KT):
                    p.op("pe", lambda e, k=k, ts=ts: e.matmul(ps_small[:, 0:NE], hf_sb[:, k, ts], rw_sb[:, k, :],
                                                              start=(k == 0), stop=(k == KT - 1)),
                         reads=[b_hf, b_const], writes=[pss_b])
                p.op("act", lambda e: e.activation(out=r_sc[:], in_=ps_small[:, 0:NE], func=AF.Sigmoid),
                     reads=[pss_b], writes=[b_r])
                R = [b_r]
                p.op("dve", lambda e: e.tensor_tensor(out=r_sel[:], in0=r_sc[:], in1=rb_sb[:], op=ALU.add), reads=R + [b_const], writes=R)
                sel3 = r_sel[:].rearrange("p (g j) -> p g j", g=4)
                t13 = r_t1[:].rearrange("p (g j) -> p g j", g=4)
                t23 = r_t2[:].rearrange("p (g j) -> p g j", g=4)
                p.op("dve", lambda e: e.tensor_reduce(out=r_g1[:], in_=sel3, axis=AX.X, op=ALU.max), reads=R, writes=R)
                p.op("dve", lambda e: e.tensor_tensor(out=t13, in0=sel3, in1=r_g1[:].unsqueeze(2).to_broadcast([128, 4, 4]),
                                                      op=ALU.is_equal), reads=R, writes=R)
                p.op("dve", lambda e: e.scalar_tensor_tensor(out=r_t2[:], in0=r_t1[:], scalar=-BIG, in1=r_sel[:],
                                                             op0=ALU.mult, op1=ALU.add), reads=R, writes=R)
                p.op("dve", lambda e: e.tensor_reduce(out=r_g2[:], in_=t23, axis=AX.X, op=ALU.max), reads=R, writes=R)
                p.op("dve", lambda e: e.tensor_tensor(out=r_g1[:], in0=r_g1[:], in1=r_g2[:], op=ALU.add), reads=R, writes=R)
                p.op("dve", lambda e: e.tensor_reduce(out=r_m[:], in_=r_g1[:], axis=AX.X, op=ALU.max), reads=R, writes=R)
                p.op("dve", lambda e: e.tensor_scalar(out=r_g2[:], in0=r_g1[:], scalar1=r_m[:, 0:1], scalar2=None,
                                                      op0=ALU.is_equal), reads=R, writes=R)
                p.op("dve", lambda e: e.tensor_scalar(out=r_g2[:], in0=r_g2[:], scalar1=-1.0, scalar2=BIG,
                                                      op0=ALU.add, op1=ALU.mult), reads=R, writes=R)
                p.op("dve", lambda e: e.tensor_tensor(out=sel3, in0=sel3, in1=r_g2[:].unsqueeze(2).to_broadcast([128, 4, 4]),
                                                      op=ALU.add), reads=R, writes=R)
                p.op("dve", lambda e: e.tensor_reduce(out=r_m[:], in_=r_sel[:], axis=AX.X, op=ALU.max), reads=R, writes=R)
                p.op("dve", lambda e: e.tensor_scalar(out=r_t1[:], in0=r_sel[:], scalar1=r_m[:, 0:1], scalar2=None,
                                                      op0=ALU.is_equal), reads=R, writes=R)
                p.op("dve", lambda e: e.scalar_tensor_tensor(out=r_sel[:], in0=r_t1[:], scalar=-BIG, in1=r_sel[:],
                                                             op0=ALU.mult, op1=ALU.add), reads=R, writes=R)
                p.op("dve", lambda e: e.tensor_reduce(out=r_m[:], in_=r_sel[:], axis=AX.X, op=ALU.max), reads=R, writes=R)
                p.op("dve", lambda e: e.tensor_scalar(out=r_t2[:], in0=r_sel[:], scalar1=r_m[:, 0:1], scalar2=None,
                                                      op0=ALU.is_equal), reads=R, writes=R)
                p.op("dve", lambda e: e.tensor_tensor(out=r_t1[:], in0=r_t1[:], in1=r_t2[:], op=ALU.add), reads=R, writes=R)
                p.op("dve", lambda e: e.tensor_tensor(out=r_t1[:], in0=r_t1[:], in1=r_sc[:], op=ALU.mult), reads=R, writes=R)
                p.op("dve", lambda e: e.tensor_reduce(out=r_m[:], in_=r_t1[:], axis=AX.X, op=ALU.add), reads=R, writes=R)
                p.op("dve", lambda e: e.reciprocal(out=r_m[:], in_=r_m[:]), reads=R, writes=R)
                p.op("dve", lambda e: e.tensor_scalar(out=r_cmb[:], in0=r_t1[:], scalar1=r_m[:, 0:1], scalar2=None,
                                                      op0=ALU.mult), reads=R, writes=R)
                p.op("pe", lambda e: e.matmul(ps_small[0:NE, 128:256], r_cmb[:], ident_sb[:], start=True, stop=True),
                     reads=R + [b_const], writes=[pss_b])
                p.op("dve", lambda e, ts=ts: e.tensor_copy(out=r_cT[:, ts], in_=ps_small[0:NE, 128:256]),
                     reads=[pss_b], writes=[b_rcT])
            p.dma("sp", combT.ap(), r_cT[:], reads=[b_rcT], writes=[b_combT])

            for e_ in range(NE):
                r_own, h_own = e_ // 2, e_ % 2
                cs = e_ % 2
                p.dma("sp", cb_sb[cs][:], combT.ap()[e_:e_ + 1, :].to_broadcast([128, TC]), reads=[b_combT], writes=[b_cb[cs]])
                hs = e_ % 2
                for half in range(2):
                    f0 = half * 512
                    loads = []
                    for (g, bg) in ((g1, bg1), (g2, bg2)):
                        s = nload % 2
                        nload += 1
                        wv = wbuf[s][:].rearrange("p (k n) -> p k n", k=KT)
                        p.dma_k("sp", wv, g.ap()[h_own * 32:(h_own + 1) * 32, r_own, :, f0:f0 + 512].rearrange("c j f -> j c f"), KT, 4,
                                reads=[bg], writes=[wbb[s]])
                        loads.append((wv, wbb[s]))
                    for ft in range(4):
                        fi = half * 4 + ft
                        pi = fi % 2
                        for k in range(KT):
                            p.op("pe", lambda e, k=k, ft=ft, pi=pi: e.matmul(
                                ps_g[pi][:], loads[0][0][:, k, ft * 128:(ft + 1) * 128], h_bf[:, k, :],
                                start=(k == 0), stop=(k == KT - 1)), reads=[loads[0][1], b_hbf], writes=[b_psg[pi]])
                        for k in range(KT):
                            p.op("pe", lambda e, k=k, ft=ft, pi=pi: e.matmul(
                                ps_u[pi][:], loads[1][0][:, k, ft * 128:(ft + 1) * 128], h_bf[:, k, :],
                                start=(k == 0), stop=(k == KT - 1)), reads=[loads[1][1], b_hbf], writes=[b_psu[pi]])
                        p.op("act", lambda e, pi=pi: e.activation(out=sg_sb[:], in_=ps_g[pi][:], func=AF.Silu),
                             reads=[b_psg[pi]], writes=[b_sg])
                        p.op("dve", lambda e, pi=pi: e.tensor_tensor(out=sg_sb[:], in0=sg_sb[:], in1=ps_u[pi][:], op=ALU.mult),
                             reads=[b_sg, b_psu[pi]], writes=[b_sg])
                        p.op("dve", lambda e, fi=fi: e.tensor_tensor(out=hid_sb[hs][:, fi, :], in0=sg_sb[:], in1=cb_sb[cs][:], op=ALU.mult),
                             reads=[b_sg, b_cb[cs]], writes=[b_hid[hs]])
                for dh in range(2):
                    d0 = dh * 2048
                    s = nload % 2
                    nload += 1
                    wv = wbuf[s][:].rearrange("p (f n) -> p f n", f=8)
                    for c4 in range(4):
                        srcap = g3.ap()[h_own * 32 + c4:(h_own + 1) * 32:4, r_own, :, d0:d0 + 2048].rearrange("f j d -> j f d")
                        p.dma("sp", wv[c4 * 32:(c4 + 1) * 32], srcap, reads=[bg3], writes=[wbb[s]])
                    for dt_ in range(16):
                        di = dh * 16 + dt_
                        pi = di % 2
                        for f in range(8):
                            p.op("pe", lambda e, f=f, dt_=dt_, pi=pi: e.matmul(
                                ps_d[pi][:], wv[:, f, dt_ * 128:(dt_ + 1) * 128], hid_sb[hs][:, f, :],
                                start=(f == 0), stop=(f == 7)), reads=[wbb[s], b_hid[hs]], writes=[b_psd[pi]])
                        if e_ == 0:
                            p.op("dve", lambda e, di=di, pi=pi: e.tensor_copy(out=hf_sb[:, di, :], in_=ps_d[pi][:]),
                                 reads=[b_psd[pi]], writes=[b_hf])
                        else:
                            p.op("dve", lambda e, di=di, pi=pi: e.tensor_tensor(out=hf_sb[:, di, :], in0=hf_sb[:, di, :],
                                                                                in1=ps_d[pi][:], op=ALU.add),
                                 reads=[b_psd[pi], b_hf], writes=[b_hf])
            for k in range(KT):
                p.op("dve", lambda e, k=k: e.scalar_tensor_tensor(out=x_sb[:, k, :], in0=hf_sb[:, k, :], scalar=G_sb[:, k:k + 1],
                                                                  in1=x_sb[:, k, :], op0=ALU.mult, op1=ALU.add),
                     reads=[b_hf, b_mod, b_x], writes=[b_x])
            p.dma_k("sp", dst.ap().rearrange("(k p) n -> p k n", p=128)[:, :, t0:t0 + TC], x_sb[:], KT, 4, reads=[b_x], writes=[dst_b])

    cur, cur_b = xT, ext
    for l in range(L):
        x1, x1_b = mix_layer(l, cur, cur_b)
        dst = yT if l == L - 1 else xmid[l]
        dst_b = p.buf('dst%d' % l)
        if do_moe:
            moe_layer(l, x1, x1_b, dst, dst_b)
        else:
            p.begin_phase()
            p.dma_k("sp", dst.ap().rearrange("(k p) n -> p k n", p=128), x1.ap().rearrange("(k p) n -> p k n", p=128), KT, 8,
                    reads=[x1_b], writes=[dst_b])
        cur, cur_b = dst, dst_b
    p.finish([dst_b])
    return p


def _prep_inputs(inputs, L, NT, ntok_total):
    x = np.asarray(inputs["x"], np.float32).reshape(-1, D)
    T = x.shape[0] // 2
    c = np.asarray(inputs["c"], np.float32)
    ada_w = np.asarray(inputs["ada_w"], np.float32)
    ada_b = np.asarray(inputs["ada_b"], np.float32)
    n2 = np.asarray(inputs["norm2_g"], np.float32)
    rw = np.ascontiguousarray(np.asarray(inputs["router_w"], np.float32))
    rb = np.asarray(inputs["router_bias"], np.float32)
    wg = np.asarray(inputs["exp_w_gate"], np.float32)
    wu = np.asarray(inputs["exp_w_up"], np.float32)
    wd = np.asarray(inputs["exp_w_down"], np.float32)
    ident = np.eye(128, dtype=np.float32)
    maps = []
    for r in range(NCORES):
        tok0 = r * NT
        b = tok0 // T
        m = {}
        m["xT"] = np.ascontiguousarray(x[tok0:tok0 + NT].T)
        m["cT"] = np.ascontiguousarray(c.T)
        sel = np.zeros((128, 2), np.float32)
        sel[:, b] = 1.0
        m["bsel"] = sel
        m["ident"] = ident
        m["ada_w"] = np.ascontiguousarray(ada_w[:L, :, r * 3072:(r + 1) * 3072])
        m["ada_b"] = np.ascontiguousarray(ada_b[:L, r * 3072:(r + 1) * 3072].reshape(L, 24, 128).transpose(0, 2, 1))
        m["norm2_g"] = np.ascontiguousarray(n2[:L].reshape(L, KT, 128).transpose(0, 2, 1))
        m["router_w"] = rw
        m["router_b"] = np.ascontiguousarray(np.broadcast_to(rb[None, :], (128, NE)))
        h = r
        TC = 256
        TB = NT * 4
        NTOT = NT * NCORES
        NQT = TB // 128
        f32 = np.float32
        m["norm1_g"] = np.ascontiguousarray(np.asarray(inputs["norm1_g"], f32)[:L].reshape(L, KT, 128).transpose(0, 2, 1))
        w_in = inputs["w_in"]
        O = dict(gq=0, gk=1024, gv=2048, gz=3072, gb=4096, ga=4104, cv=4112, cg=5136, dq=6160, dk=7184, dv=8208, gates=9232)

        def bc(v):
            v = np.asarray(v, f32).reshape(1, -1)
            return np.ascontiguousarray(np.broadcast_to(v, (128, v.shape[1])))
        for l in range(L):
            wl = np.asarray(w_in[l], f32)
            wm = np.zeros((D, 1280), f32)
            for ti, key in enumerate(("gq", "gk", "gv", "gz", "cv", "cg", "dq", "dk", "dv")):
                wm[:, ti * 128:(ti + 1) * 128] = wl[:, O[key] + h * 128:O[key] + (h + 1) * 128]
            wm[:, 1152] = wl[:, O["gb"] + h]
            wm[:, 1153] = wl[:, O["ga"] + h]
            m["w_mix%d" % l] = wm
            m["w_gat%d" % l] = np.ascontiguousarray(wl[r * 512:(r + 1) * 512, O["gates"]:])
            m["w_oa%d" % l] = np.ascontiguousarray(np.asarray(inputs["gdn_w_out"][l], f32)[r * 128:(r + 1) * 128])
            m["w_ob%d" % l] = np.ascontiguousarray(np.asarray(inputs["conf_w_out"][l], f32)[r * 128:(r + 1) * 128])
            m["w_oc%d" % l] = np.ascontiguousarray(np.asarray(inputs["diff_w_out"][l], f32)[r * 128:(r + 1) * 128])
            m["w_oo%d" % l] = np.ascontiguousarray(np.asarray(inputs["w_o"][l], f32)[r * 512:(r + 1) * 512])
        gcw = np.asarray(inputs["gdn_conv_w"], f32)[:L]
        gconv = np.zeros((L, 128, 12), f32)
        for w_ in range(3):
            for j in range(4):
                gconv[:, :, w_ * 4 + j] = gcw[:, j, w_ * 1024 + h * 128:w_ * 1024 + (h + 1) * 128]
        m["gconv"] = gconv
        gs = np.zeros((L, 128, 2), f32)
        gs[:, :, 0] = np.asarray(inputs["gdn_a_log"], f32)[:L, h][:, None]
        gs[:, :, 1] = np.asarray(inputs["gdn_dt_bias"], f32)[:L, h][:, None]
        m["gscal"] = gs
        m["gng"] = np.stack([bc(inputs["gdn_norm_g"][l]) for l in range(L)])
        m["cdw"] = np.ascontiguousarray(np.asarray(inputs["conf_dw_w"], f32)[:L, :, h * 128:(h + 1) * 128].transpose(0, 2, 1))
        m["cdb"] = np.ascontiguousarray(np.asarray(inputs["conf_dw_b"], f32)[:L, h * 128:(h + 1) * 128].reshape(L, 128, 1))
        m["clng"] = np.ascontiguousarray(np.asarray(inputs["conf_ln_g"], f32)[:L].reshape(L, 8, 128).transpose(0, 2, 1))
        m["clnb"] = np.ascontiguousarray(np.asarray(inputs["conf_ln_b"], f32)[:L].reshape(L, 8, 128).transpose(0, 2, 1))
        m["dqg"] = np.ascontiguousarray(np.tile(np.asarray(inputs["diff_q_norm_g"], f32)[:L], (1, 2)).reshape(L, 128, 1))
        m["dkg"] = np.ascontiguousarray(np.tile(np.asarray(inputs["diff_k_norm_g"], f32)[:L], (1, 2)).reshape(L, 128, 1))
        m["dlam"] = np.stack([bc(np.concatenate([np.asarray(inputs[k][l], f32) for k in
                                                 ("diff_lambda_q1", "diff_lambda_k1", "diff_lambda_q2", "diff_lambda_k2")])) for l in range(L)])
        m["dsg"] = np.stack([bc(inputs["diff_sub_g"][l]) for l in range(L)])
        slope = f32(2.0) ** f32(-(h + 1))
        pp = np.arange(128, dtype=f32)[:, None]
        dd = np.arange(NQT, dtype=f32)[None, :]
        m["abias"] = (slope * (pp - 127.0 - 128.0 * dd)).astype(f32)
        ii = np.arange(128)
        m["triu"] = (ii[:, None] <= ii[None, :]).astype(f32)
        m["trils"] = (ii[:, None] > ii[None, :]).astype(f32)
        m["blk"] = ((ii[:, None] // 64) == (ii[None, :] // 64)).astype(f32)
        s63 = np.zeros((64, 128), f32)
        s63[63, :] = 1.0
        m["sel63"] = s63
        NCHF = NT // TC
        gi = np.zeros((128, NCHF * 8), np.int32)
        for ch in range(NCHF):
            g0 = r * NT + ch * TC
            c_, tb = g0 // 512, (g0 % 512) // TC
            for hh in range(8):
                gi[:, ch * 8 + hh] = ((c_ * 8 + hh) * 128 + np.arange(128)) * (512 // TC) + tb
        m["gidx"] = gi
        rs_ = np.zeros((128, NCORES), f32)
        rs_[:, r] = 1.0
        m["rsel"] = rs_
        for l in range(L):
            m["wg%d" % l] = np.ascontiguousarray(wg[l].reshape(NE * D, FF)[r * 8192:(r + 1) * 8192])
            m["wu%d" % l] = np.ascontiguousarray(wu[l].reshape(NE * D, FF)[r * 8192:(r + 1) * 8192])
            m["wd%d" % l] = np.ascontiguousarray(wd[l].reshape(NE * FF, D)[r * 2048:(r + 1) * 2048])
        maps.append(m)
    return maps


def run(inputs, L, NT):
    p = build(L, NT)
    maps = _prep_inputs(inputs, L, NT, NT * NCORES)
    res = run_bass_kernel_spmd(p.nc, maps, core_ids=list(range(NCORES)))
    out = np.concatenate([res.results[r]["yT"].T for r in range(NCORES)], axis=0)
    return out


def kernel(**inputs):
    x = inputs["x"]
    B, T, _ = x.shape
    NT = B * T // NCORES
    out = run(inputs, 2, NT)
    return out.reshape(B, T, D).astype(np.float32)
```
